# Optimizing a Trainium2 kernel written in Bass

```python
import math
import jax, jax.numpy as jnp
from jax import lax
import numpy as np

D_MODEL = 1024
BATCH = 32
SEQ = 2048
DEPTH = 1

MOBA_HEADS = 8
MOBA_HEAD_DIM = 64
MOBA_BLOCK = 256
MOBA_TOPK = 3
MOBA_Q_CHUNK = 16
RET_HEADS = 8
RET_QK_DIM = 64
RET_V_DIM = 128
RET_CHUNK = 128
D_FF = 4 * D_MODEL
ROPE_THETA = 10000.0
EPS = 1e-6

MOBA_W = MOBA_HEADS * MOBA_HEAD_DIM
RET_QK_W = RET_HEADS * RET_QK_DIM
RET_V_W = RET_HEADS * RET_V_DIM
IN_SPLITS = (MOBA_W, MOBA_W, MOBA_W, RET_QK_W, RET_QK_W, RET_V_W, RET_V_W, D_MODEL, D_MODEL)
IN_WIDTH = 3 * MOBA_W + 2 * RET_QK_W + 2 * RET_V_W + 2 * D_MODEL
N_ADA = 6

kernel_name = "hybrid_moba_retention_adaln_block"


def rms_norm(x, w):
    xf = x.astype(jnp.float32)
    y = xf * lax.rsqrt(jnp.mean(xf * xf, axis=-1, keepdims=True) + EPS)
    return (y * w.astype(jnp.float32)).astype(x.dtype)


def modulate(h, shift, scale):
    return h * (1 + scale) + shift


def rotary_tables(seq, inv_freq):
    pos = jnp.arange(seq, dtype=jnp.float32)
    ang = pos[:, None] * inv_freq[None, :]
    return jnp.cos(ang), jnp.sin(ang)


def apply_rotary(t, cos, sin):
    t1, t2 = jnp.split(t, 2, axis=-1)
    c = cos.astype(t.dtype)
    s = sin.astype(t.dtype)
    return jnp.concatenate([t1 * c - t2 * s, t1 * s + t2 * c], axis=-1)


def to_heads(t, n_heads):
    b, s, _ = t.shape
    return t.reshape(b, s, n_heads, -1).transpose(0, 2, 1, 3)


def from_heads(t):
    b, h, s, d = t.shape
    return t.transpose(0, 2, 1, 3).reshape(b, s, h * d)


def moba_attention(q, k, v):
    B, H, S, dh = q.shape
    T = MOBA_BLOCK
    Qc = MOBA_Q_CHUNK
    nb = -(-S // T)
    pad = nb * T - S
    kp = jnp.pad(k, ((0, 0), (0, 0), (0, pad), (0, 0)))
    vp = jnp.pad(v, ((0, 0), (0, 0), (0, pad), (0, 0)))
    kb = kp.reshape(B, H, nb, T, dh)
    vb = vp.reshape(B, H, nb, T, dh)
    scale = dh ** -0.5
    n_sel = min(MOBA_TOPK, nb - 1)
    nq = S // Qc
    q_c = q.reshape(B, H, nq, Qc, dh).transpose(2, 0, 1, 3, 4)

    if n_sel > 0:
        q_blk = jnp.arange(S) // T
        k_mean = jnp.mean(kb.astype(jnp.float32), axis=3)
        gate = jnp.einsum('bhsd,bhnd->bhsn', q.astype(jnp.float32), k_mean)
        past = jnp.arange(nb)[None, :] < q_blk[:, None]
        gate = jnp.where(past[None, None], gate, -jnp.inf)
        sel_score, sel_idx = lax.top_k(gate, n_sel)
        sel_valid = jnp.isfinite(sel_score)
        idx_c = sel_idx.reshape(B, H, nq, Qc, n_sel).transpose(2, 0, 1, 3, 4)
        valid_c = sel_valid.reshape(B, H, nq, Qc, n_sel).transpose(2, 0, 1, 3, 4)
    else:
        idx_c = jnp.zeros((nq, B, H, Qc, 1), jnp.int32)
        valid_c = jnp.zeros((nq, B, H, Qc, 1), bool)

    gather_blocks = jax.vmap(jax.vmap(lambda blocks, idx: blocks[idx]))

    def one_chunk(args):
        ci, q_i, idx_i, valid_i = args
        start = ci * Qc
        blk_start = (start // T) * T
        k_own = lax.dynamic_slice_in_dim(kp, blk_start, T, axis=2)
        v_own = lax.dynamic_slice_in_dim(vp, blk_start, T, axis=2)
        qpos = start + jnp.arange(Qc)
        kpos = blk_start + jnp.arange(T)
        s_own = jnp.einsum('bhqd,bhtd->bhqt', q_i, k_own).astype(jnp.float32) * scale
        s_own = jnp.where(kpos[None, :] <= qpos[:, None], s_own, -jnp.inf)
        if n_sel == 0:
            p_own = jax.nn.softmax(s_own, axis=-1).astype(v.dtype)
            return jnp.einsum('bhqt,bhtd->bhqd', p_own, v_own)
        k_sel = gather_blocks(kb, idx_i)
        v_sel = gather_blocks(vb, idx_i)
        s_sel = jnp.einsum('bhqd,bhqntd->bhqnt', q_i, k_sel).astype(jnp.float32) * scale
        s_sel = jnp.where(valid_i[..., None], s_sel, -jnp.inf)
        s_all = jnp.concatenate([s_sel.reshape(B, H, Qc, n_sel * T), s_own], axis=-1)
        p = jax.nn.softmax(s_all, axis=-1).astype(v.dtype)
        p_sel = p[..., :n_sel * T].reshape(B, H, Qc, n_sel, T)
        p_own = p[..., n_sel * T:]
        return (jnp.einsum('bhqnt,bhqntd->bhqd', p_sel, v_sel)
                + jnp.einsum('bhqt,bhtd->bhqd', p_own, v_own))

    out = lax.map(one_chunk, (jnp.arange(nq), q_c, idx_c, valid_c))
    return out.transpose(1, 2, 0, 3, 4).reshape(B, H, S, dh)


def retention_chunkwise(q, k, v):
    B, H, S, dk = q.shape
    dv = v.shape[-1]
    C = RET_CHUNK
    nc = S // C
    f32 = jnp.float32
    q = q.astype(f32)
    k = k.astype(f32) * (dk ** -0.5)
    v = v.astype(f32)
    log_g = jnp.log(1.0 - jnp.power(2.0, -5.0 - jnp.arange(H, dtype=f32)))
    qc = q.reshape(B, H, nc, C, dk)
    kc = k.reshape(B, H, nc, C, dk)
    vc = v.reshape(B, H, nc, C, dv)
    i = jnp.arange(C, dtype=f32)
    diff = i[:, None] - i[None, :]
    decay = jnp.where(diff >= 0, jnp.exp(jnp.maximum(diff, 0.0)[None] * log_g[:, None, None]), 0.0)
    scores = jnp.einsum('bhnid,bhnjd->bhnij', qc, kc) * decay[None, :, None]
    o_intra = jnp.einsum('bhnij,bhnje->bhnie', scores, vc)
    zeta = jnp.exp((C - 1 - i)[None, :] * log_g[:, None])
    kv = jnp.einsum('bhnjd,bhnje->bhnde', kc * zeta[None, :, None, :, None], vc)
    chunk_decay = jnp.exp(C * log_g)[None, :, None, None]

    def step(state, kv_n):
        return state * chunk_decay + kv_n, state

    _, r_prev = lax.scan(step, jnp.zeros((B, H, dk, dv), f32), kv.transpose(2, 0, 1, 3, 4))
    r_prev = r_prev.transpose(1, 2, 0, 3, 4)
    xi = jnp.exp((i + 1)[None, :] * log_g[:, None])
    o_cross = jnp.einsum('bhnid,bhnde->bhnie', qc, r_prev) * xi[None, :, None, :, None]
    return (o_intra + o_cross).reshape(B, H, S, dv)


def head_group_norm(o, gain):
    mu = jnp.mean(o, axis=-1, keepdims=True)
    var = jnp.mean(jnp.square(o - mu), axis=-1, keepdims=True)
    y = (o - mu) * lax.rsqrt(var + EPS)
    return from_heads(y) * gain.astype(jnp.float32)


def setup_inputs(seed: int = 0) -> dict:
    key = jax.random.key(seed)
    ks = jax.random.split(key, 16)
    f32 = jnp.float32

    def nrm(k, shape, fan_in):
        return jax.random.normal(k, shape, f32) * (fan_in ** -0.5)

    return {
        "x": jax.random.normal(ks[0], (BATCH, SEQ, D_MODEL), f32),
        "c": jax.random.normal(ks[1], (BATCH, D_MODEL), f32),
        "ln1_w": 1.0 + 0.02 * jax.random.normal(ks[2], (DEPTH, D_MODEL), f32),
        "ln2_w": 1.0 + 0.02 * jax.random.normal(ks[3], (DEPTH, D_MODEL), f32),
        "w_ada": nrm(ks[4], (DEPTH, D_MODEL, N_ADA * D_MODEL), D_MODEL),
        "b_ada": 0.02 * jax.random.normal(ks[5], (DEPTH, N_ADA * D_MODEL), f32),
        "w_in": nrm(ks[6], (DEPTH, D_MODEL, IN_WIDTH), D_MODEL),
        "ret_gn_w": 1.0 + 0.02 * jax.random.normal(ks[7], (DEPTH, RET_V_W), f32),
        "w_moba_o": nrm(ks[8], (DEPTH, MOBA_W, D_MODEL), MOBA_W),
        "w_ret_o": nrm(ks[9], (DEPTH, RET_V_W, D_MODEL), RET_V_W),
        "w_out": nrm(ks[10], (DEPTH, D_MODEL, D_MODEL), D_MODEL),
        "w_ff1": nrm(ks[11], (DEPTH, D_MODEL, D_FF), D_MODEL),
        "w_ff2": nrm(ks[12], (DEPTH, D_FF, D_MODEL), D_FF),
        "final_norm_w": 1.0 + 0.02 * jax.random.normal(ks[13], (D_MODEL,), f32),
    }


def reference(x, c, ln1_w, ln2_w, w_ada, b_ada, w_in, ret_gn_w, w_moba_o, w_ret_o,
              w_out, w_ff1, w_ff2, final_norm_w):
    S = x.shape[1]
    rope_inv = 1.0 / (ROPE_THETA ** (jnp.arange(0, MOBA_HEAD_DIM, 2, dtype=jnp.float32) / MOBA_HEAD_DIM))
    moba_cos, moba_sin = rotary_tables(S, rope_inv)
    ret_inv = 1.0 / (ROPE_THETA ** jnp.linspace(0.0, 1.0, RET_QK_DIM // 2, dtype=jnp.float32))
    ret_cos, ret_sin = rotary_tables(S, ret_inv)
    split_at = [int(v) for v in np.cumsum(IN_SPLITS)[:-1]]
    c_act = jax.nn.silu(c)

    for l in range(DEPTH):
        ada = (c_act @ w_ada[l] + b_ada[l])[:, None, :]
        sh1, sc1, g1, sh2, sc2, g2 = jnp.split(ada, N_ADA, axis=-1)

        h = modulate(rms_norm(x, ln1_w[l]), sh1, sc1)
        proj = h @ w_in[l]
        mq, mk, mv, rq, rk, rv, rg, ga, gr = jnp.split(proj, split_at, axis=-1)

        mq = apply_rotary(to_heads(mq, MOBA_HEADS), moba_cos, moba_sin)
        mk = apply_rotary(to_heads(mk, MOBA_HEADS), moba_cos, moba_sin)
        a_out = moba_attention(mq, mk, to_heads(mv, MOBA_HEADS))
        y_a = from_heads(a_out) @ w_moba_o[l]

        rq = apply_rotary(to_heads(rq, RET_HEADS), ret_cos, ret_sin)
        rk = apply_rotary(to_heads(rk, RET_HEADS), ret_cos, ret_sin)
        r_out = retention_chunkwise(rq, rk, to_heads(rv, RET_HEADS))
        r_out = head_group_norm(r_out, ret_gn_w[l]).astype(x.dtype)
        y_r = (jax.nn.silu(rg) * r_out) @ w_ret_o[l]

        merged = jax.nn.sigmoid(ga) * y_a + jax.nn.sigmoid(gr) * y_r
        x = x + g1 * (merged @ w_out[l])

        h2 = modulate(rms_norm(x, ln2_w[l]), sh2, sc2)
        x = x + g2 * (jnp.square(jax.nn.relu(h2 @ w_ff1[l])) @ w_ff2[l])

    return rms_norm(x, final_norm_w)
```

```python
import math
import numpy as np
import concourse.bass as bass
import concourse.mybir as mybir
from concourse.bass_utils import run_bass_kernel_spmd
from contextlib import ExitStack

F32 = mybir.dt.float32
BF16 = mybir.dt.bfloat16
AF = mybir.ActivationFunctionType
ALU = mybir.AluOpType
AX = mybir.AxisListType

S = 2048
D = 1024
NCORES = 8
EPS = 1e-6
BIG = 30000.0
LN1E4 = math.log(10000.0)


class Tok:
    __slots__ = ("sem", "val", "eng")

    def __init__(self, sem, val, eng):
        self.sem = sem
        self.val = val
        self.eng = eng


class Prog:
    ENG = ["pe", "act", "dve", "pool", "sp"]
    CENG = ["pe", "act", "dve", "pool"]

    def __init__(self, nc, es):
        self.nc = nc
        self.q = {e: [] for e in self.ENG}
        self.sem = {e: es.enter_context(nc.semaphore("s_" + e)) for e in self.ENG}
        self.cnt = {e: 0 for e in self.ENG}
        self.last = {e: None for e in self.ENG}
        self.waited = {e: {} for e in self.ENG}
        self.res = {}
        self.pending = {e: [] for e in self.ENG}
        self.dsem = {}
        for e, n in (("sp", 28), ("pool", 12)):
            self.dsem[e] = [[es.enter_context(nc.semaphore("d_%s_%d" % (e, i))), 0, None] for i in range(n)]
        self.dnext = {e: 0 for e in self.dsem}
        self.dma_since_bar = []
        self.bar_toks = []
        self.nins = 0

    def _emit_wait(self, eng, tok):
        if tok is None:
            return
        assert tok.val is not None, "dependency on op whose completion inc is still pending"
        key = id(tok.sem)
        if self.waited[eng].get(key, 0) >= tok.val:
            return
        self.waited[eng][key] = tok.val
        self.q[eng].append(("wait_ge", (tok.sem, tok.val), {}, None))

    def _deps(self, eng, reads, writes):
        deps = []
        for k in reads:
            r = self.res.get(k)
            if r and r["w"] is not None:
                if not (r["w"].eng == eng and eng == "pe"):
                    deps.append(r["w"])
            if r and (k == "tpb" or (isinstance(k, tuple) and k[0] == "bank")):
                for e2, t in r["r"].items():
                    if e2 != eng:
                        deps.append(t)
        for k in writes:
            r = self.res.get(k)
            if r:
                w = r["w"]
                if w is not None and not (w.eng == eng and eng == "pe"):
                    deps.append(w)
                for e2, t in r["r"].items():
                    if e2 == eng and eng == "pe":
                        continue
                    deps.append(t)
                deps.extend(r["rd"])
        return deps

    def _record(self, tok, reads, writes, is_dma=False):
        for k in writes:
            self.res[k] = {"w": tok, "r": {}, "rd": []}
        for k in reads:
            r = self.res.setdefault(k, {"w": None, "r": {}, "rd": []})
            if is_dma:
                r["rd"].append(tok)
            else:
                r["r"][tok.eng] = tok

    def op(self, eng, name, kw, r=(), w=(), inc=True):
        for t in self._deps(eng, r, w):
            self._emit_wait(eng, t)
        self.nins += 1
        if inc:
            self.cnt[eng] += 1
            val = self.cnt[eng]
            tok = Tok(self.sem[eng], val, eng)
            self.q[eng].append((name, (), kw, (self.sem[eng], 1)))
            for pt in self.pending[eng]:
                pt.val = val
            self.pending[eng] = []
            self.last[eng] = tok
        else:
            self.q[eng].append((name, (), kw, None))
            tok = Tok(self.sem[eng], None, eng)
            self.pending[eng].append(tok)
        self._record(tok, r, w)
        return tok

    def dma(self, qeng, out, in_, r=(), w=(), after_bar=True):
        deng = "dma_" + qeng
        for t in self._deps(deng, r, w):
            self._emit_wait(qeng, t)
        if after_bar:
            for t in self.bar_toks:
                self._emit_wait(qeng, t)
        slot = self.dsem[qeng][self.dnext[qeng]]
        self.dnext[qeng] = (self.dnext[qeng] + 1) % len(self.dsem[qeng])
        if slot[2] is not None:
            self._emit_wait(qeng, slot[2])
        slot[1] += 16
        tok = Tok(slot[0], slot[1], deng)
        slot[2] = tok
        self.nins += 1
        self.q[qeng].append(("dma_start", (), dict(out=out, in_=in_), (slot[0], 16)))
        self._record(tok, r, w, is_dma=True)
        self.dma_since_bar.append(tok)
        return tok

    def barrier(self):
        for e in self.CENG:
            assert not self.pending[e], "pending incs at barrier on " + e
        toks = [self.last[e] for e in self.CENG if self.last[e] is not None]
        for e in self.CENG:
            for t in toks:
                if t.eng != e:
                    self._emit_wait(e, t)
            for t in self.dma_since_bar:
                self._emit_wait(e, t)
        self.bar_toks = toks + list(self.dma_since_bar)
        self.dma_since_bar = []

    def wait_all(self, eng, toks):
        for t in toks:
            self._emit_wait(eng, t)

    def emit(self):
        nc = self.nc

        def run(e, lst):
            for name, args, kw, inc in lst:
                ins = getattr(e, name)(*args, **kw)
                if inc is not None:
                    ins.then_inc(inc[0], inc[1])

        with nc.Block() as block:

            @block.tensor
            def _(e):
                run(e, self.q["pe"])

            @block.scalar
            def _(e):
                run(e, self.q["act"])

            @block.vector
            def _(e):
                run(e, self.q["dve"])

            @block.gpsimd
            def _(e):
                run(e, self.q["pool"])

            @block.sync
            def _(e):
                run(e, self.q["sp"])


def w_in_blocks():
    blks = {}
    for c in range(4):
        blks["M%d" % c] = [(c * 128, 128), (512 + c * 128, 128), (1024 + c * 128, 128)]
        blks["R%d" % c] = [(1536 + c * 128, 128), (2048 + c * 128, 128), (2560 + c * 256, 256)]
        blks["G%d" % c] = [(3584 + c * 256, 256)]
    for i in range(2):
        blks["GA%d" % i] = [(4608 + i * 512, 512)]
        blks["GR%d" % i] = [(5632 + i * 512, 512)]
    return blks


class _Stop(Exception):
    pass


def build(NBL, dbg=False, stop_after=None):
    nc = bass.Bass("TRN2", target_bir_lowering=False)

    def din(name, shape, dt=F32):
        return nc.dram_tensor(name, shape, dt, kind="ExternalInput").ap()

    xT = din("xT", [NBL, D, S])
    cT = din("cT", [128, 8, NBL])
    w_ada = din("w_ada", [D, 6 * D])
    badaT = din("badaT", [128, 48])
    vecs = din("vecs", [128, 32])
    w_in = din("w_in", [D, 6656])
    w_moba_o = din("w_moba_o", [512, D])
    w_ret_o = din("w_ret_o", [D, D])
    w_out = din("w_out", [D, D])
    w_ff1 = din("w_ff1", [D, 4 * D])
    w_ff2 = din("w_ff2", [4 * D, D])
    cstb = din("cstb", [128, 2816])
    cstf = din("cstf", [128, 904])
    posr = din("posr", [128, S])
    outT = nc.dram_tensor("outT", [NBL, D, S], F32, kind="ExternalOutput").ap()
    dbgs = {}
    if dbg:
        for nm, shp in (("d_h", [D, S]), ("d_q", [128, S]), ("d_k", [128, S]), ("d_a", [512, S]), ("d_g", [D, S]),
                        ("d_m", [D, S]), ("d_rq", [128, S])):
            dbgs[nm] = nc.dram_tensor(nm, shp, BF16, kind="ExternalOutput").ap()
        dbgs["d_ada"] = nc.dram_tensor("d_ada", [128, 48 * NBL], F32, kind="ExternalOutput").ap()
        dbgs["d_x1"] = nc.dram_tensor("d_x1", [D, 512], F32, kind="ExternalOutput").ap()

    def dscr(name, shape, dt=BF16):
        return nc.dram_tensor(name, shape, dt, kind="Internal").ap()

    inblks = w_in_blocks()
    wscr = {}
    for nm, pieces in inblks.items():
        nb = sum(p[1] for p in pieces)
        wscr[nm] = dscr("wb_" + nm, [128, 8, nb])
    wscr["MO"] = dscr("wb_MO", [128, 4, 1024])
    for i in range(2):
        wscr["RO%d" % i] = dscr("wb_RO%d" % i, [128, 8, 512])
        wscr["WO%d" % i] = dscr("wb_WO%d" % i, [128, 8, 512])
    for i in range(8):
        wscr["F1_%d" % i] = dscr("wb_F1_%d" % i, [128, 8, 512])
        wscr["F2_%d" % i] = dscr("wb_F2_%d" % i, [128, 32, 128])
    tabs = dscr("tabs", [4, 128, S], F32)

    with ExitStack() as es:
        P = Prog(nc, es)

        def sb(name, shape, dt):
            return es.enter_context(nc.sbuf_tensor(name, shape, dt))

        def ps(name, shape, dt):
            return es.enter_context(nc.psum_tensor(name, shape, dt))

        cb = sb("cb", [128, 2816], BF16)
        identb = cb[:, 0:128]
        pswb = cb[:, 128:256]
        B01b = cb[:, 256:768]
        Eselb = cb[:, 768:2816]
        cf = sb("cf", [128, 904], F32)
        tri = cf[:, 0:128]
        pastbias = cf[:, 128:384]
        past01 = cf[:, 384:640]
        own01 = cf[:, 640:896]
        idxc = cf[:, 896:904]
        onesb = sb("onesb", [128, 128], BF16)
        o128b = sb("o128b", [128, 128], BF16)
        cmatb = sb("cmatb", [128, 128], BF16)
        vec = sb("vec", [128, 32], F32)
        bada = sb("bada", [128, 48], F32)
        adaT = sb("adaT", [128, 48, NBL], F32)
        modv = sb("modv", [128, NBL, 16], F32)
        fn32 = sb("fn32", [128, 8], F32)
        smallc = sb("smallc", [128, 24], F32)
        cact = sb("cact", [128, 8, NBL], F32)
        wring = sb("wring", [128, 4, 4096], BF16)
        regA = sb("regA", [128, 16384], BF16)
        regB = sb("regB", [128, 24576], BF16)
        regC = sb("regC", [128, 32768], BF16)

        banks = [ps("pb%d" % i, [128, 512], F32) for i in range(7)]
        tpb = ps("tpb", [128, 1024], BF16)
        brot = {}

        def bank(role, ids):
            i = brot.get(role, 0)
            brot[role] = i + 1
            b = ids[i % len(ids)]
            return banks[b], ("bank", b)

        wr = {"i": 0}

        def load_w(nm):
            slot = wr["i"] % 4
            wr["i"] += 1
            src = wscr[nm]
            kc, nb = src.shape[1], src.shape[2]
            dst = wring[:, slot, 0:kc * nb].rearrange("p (k n) -> p k n", n=nb)
            P.dma("sp", dst, src, r=[("wscr", nm)], w=[("wring", slot)], after_bar=False)
            return dst, ("wring", slot)

        P.dma("pool", cb[:], cstb, w=["cb"])
        P.dma("sp", cf[:], cstf, w=["cf"])
        P.dma("sp", vec[:], vecs, w=["vec"])
        P.dma("sp", bada[:], badaT, w=["bada"])
        P.dma("sp", cact[:], cT, w=["cact"])

        def conv(nm, wsrc, pieces):
            srcv = wsrc.rearrange("(k p) n -> p k n", p=128)
            off = 0
            nk = srcv.shape[1]
            for (c0, n) in pieces:
                for k0 in range(0, nk, 8):
                    P.dma("pool", wscr[nm][:, k0:min(k0 + 8, nk), off:off + n], srcv[:, k0:min(k0 + 8, nk), c0:c0 + n], w=[("wscrp", nm, off, k0)])
                off += n
            P.res[("wscr", nm)] = {"w": None, "r": {}, "rd": []}
            P._wscr_parts = getattr(P, "_wscr_parts", {})
            P._wscr_parts[nm] = [("wscrp", nm, o, k0) for o in np.cumsum([0] + [p[1] for p in pieces[:-1]]).tolist()
                                 for k0 in range(0, nk, 8)]

        conv_order = []
        for c in range(4):
            conv_order.append(("M%d" % c, w_in, inblks["M%d" % c]))
        for c in range(4):
            conv_order.append(("R%d" % c, w_in, inblks["R%d" % c]))
            conv_order.append(("G%d" % c, w_in, inblks["G%d" % c]))
        conv_order.append(("MO", w_moba_o, [(0, 1024)]))
        for i in range(2):
            conv_order.append(("GA%d" % i, w_in, inblks["GA%d" % i]))
        for i in range(2):
            conv_order.append(("RO%d" % i, w_ret_o, [(i * 512, 512)]))
            conv_order.append(("GR%d" % i, w_in, inblks["GR%d" % i]))
        for i in range(2):
            conv_order.append(("WO%d" % i, w_out, [(i * 512, 512)]))
        for i in range(8):
            conv_order.append(("F1_%d" % i, w_ff1, [(i * 512, 512)]))
        for i in range(8):
            conv_order.append(("F2_%d" % i, w_ff2, [(i * 128, 128)]))
        for nm, wsrc, pieces in conv_order:
            conv(nm, wsrc, pieces)

        def load_w(nm):
            slot = wr["i"] % 4
            wr["i"] += 1
            src = wscr[nm]
            kc, nb = src.shape[1], src.shape[2]
            dst = wring[:, slot, 0:kc * nb].rearrange("p (k n) -> p k n", n=nb)
            P.dma("sp", dst, src, r=P._wscr_parts[nm], w=[("wring", slot)], after_bar=False)
            return dst, ("wring", slot)

        P.op("pool", "memset", dict(ap=onesb[:], constant=1.0), w=["onesb"])
        P.op("pool", "memset", dict(ap=o128b[:], constant=1.0 / 128.0), w=["o128b"])
        P.op("dve", "tensor_scalar", dict(out=cmatb[:], in0=identb, scalar1=-1.0 / 128.0, scalar2=None, op0=ALU.add),
             r=["cb"], w=["cmatb"])
        P.op("act", "activation", dict(out=smallc[:, 0:1], in_=idxc[:, 0:1], func=AF.Exp, scale=-LN1E4 / 32.0),
             r=["cf"], w=["sc0"])
        P.op("act", "activation", dict(out=smallc[:, 1:2], in_=idxc[:, 0:1], func=AF.Exp, scale=-LN1E4 / 31.0),
             r=["cf"], w=["sc1"])
        P.op("act", "activation", dict(out=smallc[:, 14:18], in_=idxc[:, 2:6], func=AF.Exp, scale=-math.log(2.0),
                                        bias=-5.0 * math.log(2.0)), r=["cf"], w=["sc14"])
        P.op("act", "activation", dict(out=smallc[:, 2:6], in_=smallc[:, 14:18], func=AF.Ln, scale=-1.0, bias=1.0),
             r=["sc14"], w=["sc2"])
        P.op("dve", "tensor_scalar", dict(out=smallc[:, 6:10], in0=smallc[:, 2:6], scalar1=-1.0, scalar2=None, op0=ALU.mult),
             r=["sc2"], w=["sc6"])
        P.op("dve", "tensor_scalar", dict(out=smallc[:, 10:14], in0=smallc[:, 2:6], scalar1=-1.0, scalar2=math.log(0.125),
                                           op0=ALU.mult, op1=ALU.add), r=["sc2"], w=["sc10"])
        P.op("dve", "tensor_scalar", dict(out=fn32[:], in0=vec[:, 16:24], scalar1=32.0, scalar2=None, op0=ALU.mult),
             r=["vec"], w=["fn32"])

        P.op("act", "activation", dict(out=cact[:], in_=cact[:], func=AF.Silu), r=["cact"], w=["cact"])
        wst = [regC[:, 0:8192].bitcast(F32).rearrange("p (k n) -> p k n", n=512),
               regC[:, 8192:16384].bitcast(F32).rearrange("p (k n) -> p k n", n=512)]
        wadav = w_ada.rearrange("(k p) n -> p k n", p=128)
        ada_ps, ada_key = banks[0], ("bank", 0)
        ada_v = ada_ps[:, 0:48 * NBL].rearrange("p (f b) -> p f b", b=NBL)
        for blk in range(12):
            st = wst[blk % 2]
            P.dma("sp", st, wadav[:, :, blk * 512:(blk + 1) * 512], w=[("wst", blk % 2)])
            for fi in range(4):
                f = blk * 4 + fi
                for k in range(8):
                    P.op("pe", "matmul", dict(out=ada_v[:, f, :], lhsT=st[:, k, fi * 128:(fi + 1) * 128], rhs=cact[:, k, :],
                                               start=(k == 0), stop=(k == 7)),
                         r=[("wst", blk % 2), "cact"], w=[("adaps", f)], inc=(k == 7))
        P.op("dve", "tensor_tensor", dict(out=adaT[:], in0=ada_v, in1=bada[:].unsqueeze(2).to_broadcast([128, 48, NBL]),
                                           op=ALU.add), r=[("adaps", f) for f in range(48)] + ["bada"], w=["adaT"])
        for b in range(NBL):
            P.op("dve", "scalar_tensor_tensor", dict(out=modv[:, b, 0:8], in0=adaT[:, 8:16, b], scalar=1.0, in1=vec[:, 0:8],
                                                      op0=ALU.add, op1=ALU.mult), r=["adaT", "vec"], w=[("modv", b, 0)])
            P.op("dve", "scalar_tensor_tensor", dict(out=modv[:, b, 8:16], in0=adaT[:, 32:40, b], scalar=1.0, in1=vec[:, 8:16],
                                                      op0=ALU.add, op1=ALU.mult), r=["adaT", "vec"], w=[("modv", b, 1)])
        P.op("dve", "tensor_scalar", dict(out=modv[:], in0=modv[:], scalar1=32.0, scalar2=None, op0=ALU.mult),
             r=[("modv", b, i) for b in range(NBL) for i in range(2)], w=["modv"])
        if dbg:
            P.dma("sp", dbgs["d_ada"], adaT[:].rearrange("p f b -> p (f b)"), r=["adaT"])

        posS = regC[:, 16384:20480].bitcast(F32)
        tA = regC[:, 20480:24576].bitcast(F32)
        tB = regC[:, 24576:28672].bitcast(F32)
        P.dma("sp", posS, posr, w=["posS"])
        TWO_PI = 2.0 * math.pi
        MAGIC = 12582912.0
        tK = regC[:, 28672:32768].bitcast(F32)
        for mi in range(2):
            for ti, phase in ((0, math.pi / 2), (1, 0.0)):
                P.op("dve", "tensor_scalar", dict(out=tA, in0=posS, scalar1=smallc[:, mi:mi + 1], scalar2=phase,
                                                   op0=ALU.mult, op1=ALU.add), r=["posS", "sc0", "sc1"], w=["tA"])
                P.op("dve", "tensor_scalar", dict(out=tK, in0=tA, scalar1=1.0 / TWO_PI, scalar2=MAGIC,
                                                   op0=ALU.mult, op1=ALU.add), r=["tA"], w=["tK"])
                P.op("dve", "tensor_scalar", dict(out=tK, in0=tK, scalar1=-MAGIC, scalar2=None, op0=ALU.add), r=["tK"], w=["tK"])
                P.op("dve", "scalar_tensor_tensor", dict(out=tA, in0=tK, scalar=-TWO_PI, in1=tA, op0=ALU.mult, op1=ALU.add),
                     r=["tK", "tA"], w=["tA"])
                P.op("dve", "tensor_scalar", dict(out=tA, in0=tA, scalar1=3.14159, scalar2=-3.14159, op0=ALU.min, op1=ALU.max),
                     r=["tA"], w=["tA"])
                if ti == 0:
                    P.op("act", "activation", dict(out=tB, in_=tA, func=AF.Sin), r=["tA"], w=["tB"])
                else:
                    P.op("act", "activation", dict(out=tA, in_=tA, func=AF.Sin), r=["tA"], w=["tA"])
                    P.op("dve", "tensor_scalar", dict(out=tB, in0=tA, scalar1=idxc[:, 1:2], scalar2=None, op0=ALU.mult),
                         r=["tA", "cf"], w=["tB"])
                P.dma("sp", tabs[mi * 2 + ti], tB, r=["tB"], w=[("tabs", mi * 2 + ti)])
        P.barrier()
        stage = [0]

        def chk():
            stage[0] += 1
            if stop_after is not None and stage[0] >= stop_after:
                raise _Stop()

        hT = [regA[:, c * 2048:(c + 1) * 2048] for c in range(8)]
        aT = [regB[:, c * 2048:(c + 1) * 2048] for c in range(4)]
        gT = [regB[:, 8192 + h * 2048:8192 + (h + 1) * 2048] for h in range(8)]
        merged = [regC[:, f * 2048:(f + 1) * 2048] for f in range(8)]

        def gs(g):
            return slice(g * 512, (g + 1) * 512)

        def proj_fm(pbank, pkey, wblk, wkey, c0, g, rkeys):
            for k in range(8):
                P.op("pe", "matmul", dict(out=pbank[:], lhsT=wblk[:, k, c0:c0 + 128], rhs=hT[k][:, gs(g)],
                                           start=(k == 0), stop=(k == 7)),
                     r=[wkey] + rkeys, w=[pkey], inc=(k == 7))

        try:
          for b in range(NBL):
            chk()
            a1 = modv[:, b, 0:8]
            a2 = modv[:, b, 8:16]
            sh1 = adaT[:, 0:8, b]
            g1 = adaT[:, 16:24, b]
            sh2 = adaT[:, 24:32, b]
            g2 = adaT[:, 40:48, b]
            hkeys = [("hT", k, g) for k in range(8) for g in range(4)]

            xs = [regC[:, c * 4096:(c + 1) * 4096].bitcast(F32) for c in range(8)]
            sq = [regB[:, 0:2048], regB[:, 2048:4096]]
            rstd = regB[:, 4096:8192].bitcast(F32)
            tmpn = [regB[:, 8192:12288].bitcast(F32), regB[:, 12288:16384].bitcast(F32)]
            for c in range(8):
                P.dma("sp", xs[c], xT[b, c * 128:(c + 1) * 128, :], w=[("xs", c)])
            for c in range(8):
                P.op("act", "activation", dict(out=sq[c % 2], in_=xs[c], func=AF.Square), r=[("xs", c)], w=[("sq", c % 2)])
                for g in range(4):
                    P.op("pe", "matmul", dict(out=banks[g][:], lhsT=onesb[:], rhs=sq[c % 2][:, gs(g)], start=(c == 0), stop=(c == 7)),
                         r=[("sq", c % 2), "onesb"], w=[("bank", g)], inc=(g == 3))
            for g in range(4):
                P.op("act", "activation", dict(out=rstd[:, gs(g)], in_=banks[g][:], func=AF.Ln, bias=1024.0 * EPS, scale=1.0),
                     r=[("bank", g)], w=[("rstd", g)])
                P.op("act", "activation", dict(out=rstd[:, gs(g)], in_=rstd[:, gs(g)], func=AF.Exp, scale=-0.5),
                     r=[("rstd", g)], w=[("rstd", g)])
            for c in range(8):
                P.op("dve", "scalar_tensor_tensor", dict(out=tmpn[c % 2], in0=xs[c], scalar=a1[:, c:c + 1], in1=rstd,
                                                          op0=ALU.mult, op1=ALU.mult),
                     r=[("xs", c), "modv"] + [("rstd", g) for g in range(4)], w=[("tmpn", c % 2)])
                P.op("act", "activation", dict(out=hT[c], in_=tmpn[c % 2], func=AF.Identity, bias=sh1[:, c:c + 1], scale=1.0),
                     r=[("tmpn", c % 2), "adaT"], w=[("hT", c, g) for g in range(4)])
            if dbg and b == 0:
                for c in range(8):
                    P.dma("sp", dbgs["d_h"][c * 128:(c + 1) * 128, :], hT[c], r=[("hT", c, 0)])
            P.barrier()
            chk()

            cosm = regC[:, 0:4096].bitcast(F32)
            sinm = regC[:, 4096:8192].bitcast(F32)
            qT = regC[:, 8192:10240]
            kT = regC[:, 10240:12288]
            vaug = regC[:, 12288:16384].rearrange("p (t h e) -> p t h e", t=16, h=2)
            q_bf = regC[:, 16384:16896]
            t1 = regC[:, 16896:17920].bitcast(F32)
            t2 = regC[:, 17920:18944].bitcast(F32)
            gm = regC[:, 18944:19456].bitcast(F32)
            cmpb = regC[:, 19456:21504]
            cnt = regC[:, 21504:22016].bitcast(F32)
            sel = regC[:, 22016:22528].bitcast(F32)
            btok = regC[:, 22528:22784]
            biasT = regC[:, 22784:24832]
            pTr = [regC[:, 24832 + i * 512:24832 + (i + 1) * 512] for i in range(3)]
            rs = regC[:, 26368:27392].bitcast(F32)
            km = regC[:, 27392:27408].bitcast(F32)
            kmb = regC[:, 27408:27416]
            P.dma("sp", cosm, tabs[0], r=[("tabs", 0)], w=["cosm"])
            P.dma("sp", sinm, tabs[1], r=[("tabs", 1)], w=["sinm"])
            P.op("dve", "memset", dict(ap=vaug[:, :, :, 64:128], constant=1.0), w=["vones"])
            P.op("dve", "memset", dict(ap=biasT, constant=0.0), w=[("biasT", g) for g in range(4)])

            def rotary(dst, dkey, wblk, wkey, c0, cosT, sinT, ckeys, decay=None):
                for g in range(4):
                    pq, pk_ = bank("mm", [0, 1])
                    proj_fm(pq, pk_, wblk, wkey, c0, g, hkeys)
                    P.op("act", "activation", dict(out=q_bf, in_=pq[:], func=AF.Copy), r=[pk_], w=["q_bf"])
                    psw, psk = bank("sw", [2, 3])
                    P.op("pe", "matmul", dict(out=psw[:], lhsT=pswb, rhs=q_bf, start=True, stop=True), r=["q_bf", "cb"], w=[psk])
                    P.op("dve", "tensor_tensor", dict(out=t1, in0=pq[:], in1=cosT[:, gs(g)], op=ALU.mult), r=[pk_] + ckeys, w=["t1"])
                    P.op("dve", "tensor_tensor", dict(out=t2, in0=psw[:], in1=sinT[:, gs(g)], op=ALU.mult), r=[psk] + ckeys, w=["t2"])
                    if decay is None:
                        P.op("dve", "tensor_tensor", dict(out=dst[:, gs(g)], in0=t1, in1=t2, op=ALU.add), r=["t1", "t2"], w=[(dkey, g)])
                    else:
                        decay(g, dst, dkey)

            for c in range(4):
                wblk, wkey = load_w("M%d" % c)
                chk()
                rotary(qT, "qT", wblk, wkey, 0, cosm, sinm, ["cosm", "sinm"])
                rotary(kT, "kT", wblk, wkey, 128, cosm, sinm, ["cosm", "sinm"])
                if dbg and b == 0 and c == 0:
                    P.dma("sp", dbgs["d_q"], qT, r=[("qT", g) for g in range(4)])
                    P.dma("sp", dbgs["d_k"], kT, r=[("kT", g) for g in range(4)])
                chk()
                for tq in range(4):
                    pv, pvk = bank("mm", [0, 1])
                    for tt in range(4):
                        t = tq * 4 + tt
                        for k in range(8):
                            P.op("pe", "matmul", dict(out=pv[:, tt * 128:(tt + 1) * 128], lhsT=hT[k][:, t * 128:(t + 1) * 128],
                                                       rhs=wblk[:, k, 256:384], start=(k == 0), stop=(k == 7)),
                                 r=[wkey] + hkeys, w=[pvk], inc=(k == 7 and tt == 3))
                    P.op("act", "activation", dict(out=vaug[:, tq * 4:(tq + 1) * 4, :, 0:64],
                                                    in_=pv[:].rearrange("p (t h e) -> p t h e", t=4, h=2), func=AF.Copy),
                         r=[pvk], w=[("vaug", tq)])
                chk()
                P.op("dve", "tensor_reduce", dict(out=km, in_=kT.rearrange("p (j t) -> p j t", t=256), axis=AX.X, op=ALU.add),
                     r=[("kT", g) for g in range(4)], w=["km"])
                P.op("dve", "tensor_scalar", dict(out=kmb, in0=km, scalar1=1.0 / 256.0, scalar2=None, op0=ALU.mult), r=["km"], w=["kmb"])
                chk()
                gpsl = [(banks[4], ("bank", 4)), (banks[5], ("bank", 5))]
                gmv = gm.rearrange("p (t h j) -> p t h j", t=16, h=2)
                pbv = pastbias.rearrange("p (t h j) -> p t h j", t=16, h=2)
                for hh in range(2):
                    gps, gk = gpsl[hh]
                    gv = gps[:, 0:128].rearrange("p (t j) -> p t j", j=8)
                    for t in range(16):
                        P.op("pe", "matmul", dict(out=gv[:, t, :], lhsT=qT[hh * 64:(hh + 1) * 64, t * 128:(t + 1) * 128],
                                                   rhs=kmb[hh * 64:(hh + 1) * 64, :], start=True, stop=True),
                             r=[("qT", t // 4), "kmb"], w=[gk], inc=(t == 15))
                    P.op("dve", "tensor_tensor", dict(out=gmv[:, :, hh, :], in0=gv, in1=pbv[:, :, hh, :], op=ALU.add),
                         r=[gk, "cf"], w=[("gm", hh)])
                chk()
                gm3 = gm.rearrange("p (g j) -> p g j", j=8)
                P.op("dve", "tensor_tensor", dict(out=cmpb.rearrange("p (g a c) -> p g a c", a=8, c=8),
                                                   in0=gm3.unsqueeze(2).to_broadcast([128, 32, 8, 8]),
                                                   in1=gm3.unsqueeze(3).to_broadcast([128, 32, 8, 8]), op=ALU.is_gt),
                     r=[("gm", 0), ("gm", 1)], w=["cmpb"])
                P.op("dve", "tensor_reduce", dict(out=cnt, in_=cmpb.rearrange("p (g c) -> p g c", c=8), axis=AX.X, op=ALU.add),
                     r=["cmpb"], w=["cnt"])
                P.op("dve", "scalar_tensor_tensor", dict(out=sel, in0=cnt, scalar=2.5, in1=past01, op0=ALU.is_lt, op1=ALU.mult),
                     r=["cnt", "cf"], w=["sel"])
                P.op("dve", "tensor_tensor", dict(out=sel, in0=sel, in1=own01, op=ALU.add), r=["sel", "cf"], w=["sel"])
                P.op("dve", "tensor_scalar", dict(out=btok, in0=sel, scalar1=-1.0, scalar2=BIG, op0=ALU.add, op1=ALU.mult),
                     r=["sel"], w=["btok"])
                chk()
                btv = btok.rearrange("p (t x) -> p t x", x=16)
                for g in range(4):
                    for tt in range(4):
                        P.op("pe", "transpose", dict(out=tpb[0:16, tt * 128:(tt + 1) * 128], in_=btv[:, g * 4 + tt, :], identity=identb),
                             r=["btok", "cb"], w=["tpb"], inc=(tt == 3))
                    P.op("act", "activation", dict(out=biasT[0:16, gs(g)], in_=tpb[0:16, 0:512], func=AF.Copy), r=["tpb"], w=[("biasT", g)])
                chk()
                pi = 0
                for hh in range(2):
                    hs = slice(hh * 64, (hh + 1) * 64)
                    for qg in range(4):
                        acc, acck = bank("acc", [5, 6])
                        full = list(range(0, 4 * qg + 2))
                        part = [4 * qg + 2, 4 * qg + 3]
                        order = [full[0]] + part + full[1:]
                        for oi, kt in enumerate(order):
                            j = kt // 2
                            c0 = 256 if j == 2 * qg + 1 else 0
                            cols = slice(c0, 512)
                            qcols = slice(qg * 512 + c0, (qg + 1) * 512)
                            diag = (j >= 2 * qg)
                            sps, spk = bank("S", [2, 3])
                            P.op("pe", "matmul", dict(out=sps[:, cols], lhsT=kT[hs, kt * 128:(kt + 1) * 128], rhs=qT[hs, qcols],
                                                       start=True, stop=False), r=[("kT", kt // 4), ("qT", qg)], w=[spk], inc=False)
                            P.op("pe", "matmul", dict(out=sps[:, cols], lhsT=Eselb[:, (hh * 8 + j) * 128:(hh * 8 + j + 1) * 128],
                                                       rhs=biasT[:, qcols], start=False, stop=(not diag)),
                                 r=[("biasT", qg), "cb"], w=[spk], inc=(not diag))
                            if diag:
                                oc = (j - 2 * qg) * 256
                                P.op("pe", "matmul", dict(out=sps[:, oc:oc + 256], lhsT=identb, rhs=B01b[:, (kt % 2) * 256:(kt % 2 + 1) * 256],
                                                           start=False, stop=True), r=["cb"], w=[spk])
                            pT = pTr[pi % 3]
                            pkey = ("pT", pi % 3)
                            pi += 1
                            P.op("act", "activation", dict(out=pT[:, cols], in_=sps[:, cols], func=AF.Exp, scale=0.125), r=[spk], w=[pkey])
                            P.op("pe", "matmul", dict(out=acc[:, cols], lhsT=vaug[:, kt, hh, :], rhs=pT[:, cols],
                                                       start=(oi == 0), stop=(oi == len(order) - 1)),
                                 r=[pkey, ("vaug", kt // 4), "vones"], w=[acck])
                        P.op("dve", "reciprocal", dict(out=rs[0:64, :], in_=acc[64:128, :]), r=[acck], w=["rs"])
                        P.op("dve", "tensor_tensor", dict(out=aT[c][hs, gs(qg)], in0=acc[0:64, :], in1=rs[0:64, :], op=ALU.mult),
                             r=[acck, "rs"], w=[("aT", c, hh, qg)])
            if dbg and b == 0:
                for c in range(4):
                    P.dma("sp", dbgs["d_a"][c * 128:(c + 1) * 128, :], aT[c], r=[("aT", c, hh, qg) for hh in range(2) for qg in range(4)])
            P.barrier()
            chk()

            cosr = regC[:, 0:4096].bitcast(F32)
            sinr = regC[:, 4096:8192].bitcast(F32)
            posR = regC[:, 8192:12288].bitcast(F32)
            qd = regC[:, 12288:14336]
            kd = regC[:, 14336:16384]
            vtok = regC[:, 16384:20480].rearrange("p (t e) -> p t e", e=256)
            q_bf = regC[:, 20480:20992]
            t1 = regC[:, 20992:22016].bitcast(F32)
            t2 = regC[:, 22016:23040].bitcast(F32)
            dq = regC[:, 23040:24064].bitcast(F32)
            dk = regC[:, 24064:25088].bitcast(F32)
            ssum = regC[:, 25088:26112].bitcast(F32)
            Smr = [regC[:, 26112 + i * 128:26112 + (i + 1) * 128] for i in range(2)]
            ktokr = [regC[:, 26368 + i * 64:26368 + (i + 1) * 64] for i in range(2)]
            Rf = [regC[:, 26496 + i * 256:26496 + (i + 1) * 256].bitcast(F32) for i in range(2)]
            Rbf = regC[:, 27008:27136]
            obf = [regC[:, 27136 + i * 512:27136 + (i + 1) * 512] for i in range(2)]
            sqb = [regC[:, 28160 + i * 512:28160 + (i + 1) * 512] for i in range(2)]
            rstdn = regC[:, 29184:30208].bitcast(F32)
            yn = regC[:, 30208:31232].bitcast(F32)
            srg = regC[:, 31232:31744]
            P.dma("sp", cosr, tabs[2], r=[("tabs", 2)], w=["cosr"])
            P.dma("sp", sinr, tabs[3], r=[("tabs", 3)], w=["sinr"])
            P.dma("sp", posR, posr, w=["posR"])
            for c in range(4):
                wblk, wkey = load_w("R%d" % c)
                gblk, gkey = load_w("G%d" % c)

                def dec_q(g, dst, dkey, c=c):
                    P.op("act", "activation", dict(out=dq, in_=posR[:, gs(g)], func=AF.Exp, scale=smallc[:, 2 + c:3 + c],
                                                    bias=smallc[:, 2 + c:3 + c]), r=["posR", "sc2"], w=["dq"])
                    P.op("dve", "tensor_tensor", dict(out=ssum, in0=t1, in1=t2, op=ALU.add), r=["t1", "t2"], w=["ssum"])
                    P.op("dve", "tensor_tensor", dict(out=dst[:, gs(g)], in0=ssum, in1=dq, op=ALU.mult), r=["ssum", "dq"], w=[(dkey, g)])

                def dec_k(g, dst, dkey, c=c):
                    P.op("act", "activation", dict(out=dk, in_=posR[:, gs(g)], func=AF.Exp, scale=smallc[:, 6 + c:7 + c],
                                                    bias=smallc[:, 10 + c:11 + c]), r=["posR", "sc6", "sc10"], w=["dk"])
                    P.op("dve", "tensor_tensor", dict(out=ssum, in0=t1, in1=t2, op=ALU.add), r=["t1", "t2"], w=["ssum"])
                    P.op("dve", "tensor_tensor", dict(out=dst[:, gs(g)], in0=ssum, in1=dk, op=ALU.mult), r=["ssum", "dk"], w=[(dkey, g)])

                rotary(qd, "qd", wblk, wkey, 0, cosr, sinr, ["cosr", "sinr"], decay=dec_q)
                rotary(kd, "kd", wblk, wkey, 128, cosr, sinr, ["cosr", "sinr"], decay=dec_k)
                if dbg and b == 0 and c == 0:
                    P.dma("sp", dbgs["d_rq"], qd, r=[("qd", g) for g in range(4)])
                for t2_ in range(8):
                    pv, pvk = bank("mm", [0, 1])
                    for tt in range(2):
                        t = t2_ * 2 + tt
                        for k in range(8):
                            P.op("pe", "matmul", dict(out=pv[:, tt * 256:(tt + 1) * 256], lhsT=hT[k][:, t * 128:(t + 1) * 128],
                                                       rhs=wblk[:, k, 256:512], start=(k == 0), stop=(k == 7)),
                                 r=[wkey] + hkeys, w=[pvk], inc=(k == 7 and tt == 1))
                    P.op("act", "activation", dict(out=vtok[:, t2_ * 2:(t2_ + 1) * 2, :],
                                                    in_=pv[:].rearrange("p (t e) -> p t e", e=256), func=AF.Copy),
                         r=[pvk], w=[("vtok", t2_)])
                P.op("dve", "memset", dict(ap=Rf[0][0:64, :], constant=0.0), w=[("Rf", 0)])
                P.op("dve", "memset", dict(ap=Rf[1][0:64, :], constant=0.0), w=[("Rf", 1)])
                ops_ = {}
                for n in range(16):
                    g = n // 4
                    ncol = slice(n * 128, (n + 1) * 128)
                    for hh in range(2):
                        h = 2 * c + hh
                        hs = slice(hh * 64, (hh + 1) * 64)
                        if n % 4 == 0:
                            ops_[hh] = bank("o", [5, 6])
                        o_ps, ok = ops_[hh]
                        ocol = slice((n % 4) * 128, (n % 4 + 1) * 128)
                        sps, spk = bank("S", [2, 3])
                        P.op("pe", "matmul", dict(out=sps[:, 0:128], lhsT=kd[hs, ncol], rhs=qd[hs, ncol], start=True, stop=True),
                             r=[("kd", g), ("qd", g)], w=[spk])
                        Sm = Smr[(n * 2 + hh) % 2]
                        smk = ("Sm", (n * 2 + hh) % 2)
                        P.op("dve", "tensor_tensor", dict(out=Sm, in0=sps[:, 0:128], in1=tri, op=ALU.mult), r=[spk, "cf"], w=[smk])
                        P.op("pe", "matmul", dict(out=o_ps[:, ocol], lhsT=vtok[:, n, hh * 128:(hh + 1) * 128], rhs=Sm, start=True, stop=(n == 0)),
                             r=[smk, ("vtok", n // 2)], w=[ok], inc=(n == 0))
                        if n > 0:
                            P.op("pe", "matmul", dict(out=o_ps[:, ocol], lhsT=Rbf[hs, :], rhs=qd[hs, ncol], start=False, stop=True),
                                 r=[("Rbf", hh), ("qd", g)], w=[ok])
                        if n < 15:
                            P.op("pe", "transpose", dict(out=tpb[:, 0:64], in_=kd[hs, ncol], identity=identb[hs, hs]),
                                 r=[("kd", g), "cb"], w=["tpb"])
                            ktok = ktokr[hh]
                            P.op("act", "activation", dict(out=ktok, in_=tpb[:, 0:64], func=AF.Copy), r=["tpb"], w=[("ktok", hh)])
                            kvp, kvk = banks[4], ("bank", 4)
                            P.op("pe", "matmul", dict(out=kvp[0:64, 0:128], lhsT=ktok, rhs=vtok[:, n, hh * 128:(hh + 1) * 128], start=True, stop=True),
                                 r=[("ktok", hh), ("vtok", n // 2)], w=[kvk])
                            P.op("dve", "tensor_tensor", dict(out=Rf[hh][0:64, :], in0=kvp[0:64, 0:128], in1=Rf[hh][0:64, :], op=ALU.add),
                                 r=[kvk, ("Rf", hh)], w=[("Rf", hh)])
                            P.op("act", "activation", dict(out=Rbf[hs, :], in_=Rf[hh][0:64, :], func=AF.Copy), r=[("Rf", hh)], w=[("Rbf", hh)])
                        if n % 4 == 3:
                            ob = obf[hh]
                            P.op("act", "activation", dict(out=ob, in_=o_ps[:], func=AF.Copy), r=[ok], w=[("obf", hh)])
                            cen, cenk = bank("mm", [0, 1])
                            P.op("pe", "matmul", dict(out=cen[:], lhsT=cmatb[:], rhs=ob, start=True, stop=True), r=[("obf", hh), "cmatb"], w=[cenk])
                            sq_ = sqb[hh]
                            P.op("act", "activation", dict(out=sq_, in_=cen[:], func=AF.Square), r=[cenk], w=[("sqb", hh)])
                            var, vark = banks[4], ("bank", 4)
                            P.op("pe", "matmul", dict(out=var[:], lhsT=o128b[:], rhs=sq_, start=True, stop=True), r=[("sqb", hh), "o128b"], w=[vark])
                            P.op("act", "activation", dict(out=rstdn, in_=var[:], func=AF.Ln, bias=EPS, scale=1.0), r=[vark], w=["rstdn"])
                            P.op("act", "activation", dict(out=rstdn, in_=rstdn, func=AF.Exp, scale=-0.5), r=["rstdn"], w=["rstdn"])
                            P.op("dve", "tensor_tensor", dict(out=yn, in0=cen[:], in1=rstdn, op=ALU.mult), r=[cenk, "rstdn"], w=["yn"])
                            prg, prgk = bank("mm", [0, 1])
                            proj_fm(prg, prgk, gblk, gkey, hh * 128, g, hkeys)
                            P.op("act", "activation", dict(out=srg, in_=prg[:], func=AF.Silu), r=[prgk], w=["srg"])
                            P.op("dve", "scalar_tensor_tensor", dict(out=gT[h][:, gs(g)], in0=yn, scalar=vec[:, 24 + h:25 + h], in1=srg,
                                                                      op0=ALU.mult, op1=ALU.mult), r=["yn", "srg", "vec"], w=[("gT", h, g)])
            if dbg and b == 0:
                for h in range(8):
                    P.dma("sp", dbgs["d_g"][h * 128:(h + 1) * 128, :], gT[h], r=[("gT", h, g) for g in range(4)])
            P.barrier()
            chk()

            sgr = [regC[:, 16384 + i * 512:16384 + (i + 1) * 512] for i in range(2)]
            tmpo = [regC[:, 17408 + i * 1024:17408 + (i + 1) * 1024].bitcast(F32) for i in range(2)]
            wmo, wmok = load_w("MO")
            it = 0
            for f in range(8):
                if f % 4 == 0:
                    gab, gabk = load_w("GA%d" % (f // 4))
                for g in range(4):
                    pga, pgak = bank("mm", [0, 1])
                    proj_fm(pga, pgak, gab, gabk, (f % 4) * 128, g, hkeys)
                    sg = sgr[it % 2]
                    P.op("act", "activation", dict(out=sg, in_=pga[:], func=AF.Sigmoid), r=[pgak], w=[("sgr", it % 2)])
                    pya, pyak = bank("S", [2, 3])
                    for cc in range(4):
                        P.op("pe", "matmul", dict(out=pya[:], lhsT=wmo[:, cc, f * 128:(f + 1) * 128], rhs=aT[cc][:, gs(g)],
                                                   start=(cc == 0), stop=(cc == 3)),
                             r=[wmok] + [("aT", cc, hh, g) for hh in range(2)], w=[pyak], inc=(cc == 3))
                    P.op("dve", "tensor_tensor", dict(out=merged[f][:, gs(g)], in0=pya[:], in1=sg, op=ALU.mult),
                         r=[pyak, ("sgr", it % 2)], w=[("merged", f, g)])
                    it += 1
            for f in range(8):
                if f % 4 == 0:
                    grb, grbk = load_w("GR%d" % (f // 4))
                    rob, robk = load_w("RO%d" % (f // 4))
                for g in range(4):
                    pga, pgak = bank("mm", [0, 1])
                    proj_fm(pga, pgak, grb, grbk, (f % 4) * 128, g, hkeys)
                    sg = sgr[it % 2]
                    P.op("act", "activation", dict(out=sg, in_=pga[:], func=AF.Sigmoid), r=[pgak], w=[("sgr", it % 2)])
                    pyr, pyrk = bank("S", [2, 3])
                    for h in range(8):
                        P.op("pe", "matmul", dict(out=pyr[:], lhsT=rob[:, h, (f % 4) * 128:(f % 4 + 1) * 128], rhs=gT[h][:, gs(g)],
                                                   start=(h == 0), stop=(h == 7)), r=[robk, ("gT", h, g)], w=[pyrk], inc=(h == 7))
                    tm = tmpo[it % 2]
                    P.op("dve", "tensor_tensor", dict(out=tm, in0=pyr[:], in1=sg, op=ALU.mult), r=[pyrk, ("sgr", it % 2)], w=[("tmpo", it % 2)])
                    P.op("dve", "tensor_tensor", dict(out=merged[f][:, gs(g)], in0=merged[f][:, gs(g)], in1=tm, op=ALU.add),
                         r=[("merged", f, g), ("tmpo", it % 2)], w=[("merged", f, g)])
                    it += 1
            if dbg and b == 0:
                for f in range(8):
                    P.dma("sp", dbgs["d_m"][f * 128:(f + 1) * 128, :], merged[f], r=[("merged", f, g) for g in range(4)])
            P.barrier()
            chk()

            x1 = regA[:, 0:8192].bitcast(F32).rearrange("p (f t) -> p f t", t=512)
            h2 = regA[:, 8192:12288].rearrange("p (f t) -> p f t", t=512)
            sq2 = [regA[:, 12288 + i * 512:12288 + (i + 1) * 512] for i in range(2)]
            rstd2 = regA[:, 13312:14336].bitcast(F32)
            tmp2 = [regA[:, 14336 + i * 1024:14336 + (i + 1) * 1024].bitcast(F32) for i in range(2)]
            u = regB[:, 0:16384].rearrange("p (f t) -> p f t", t=512)
            rl = [regB[:, 16384 + i * 512:16384 + (i + 1) * 512] for i in range(2)]
            xTv = xT[b].rearrange("(f p) t -> p f t", p=128)
            oTv = outT[b].rearrange("(f p) t -> p f t", p=128)

            def rms_stats(src_keys):
                ssp, ssk = banks[4], ("bank", 4)
                for f in range(8):
                    P.op("act", "activation", dict(out=sq2[f % 2], in_=x1[:, f, :], func=AF.Square), r=[src_keys[f]], w=[("sq2", f % 2)])
                    P.op("pe", "matmul", dict(out=ssp[:], lhsT=onesb[:], rhs=sq2[f % 2], start=(f == 0), stop=(f == 7)),
                         r=[("sq2", f % 2), "onesb"], w=[ssk])
                P.op("act", "activation", dict(out=rstd2, in_=ssp[:], func=AF.Ln, bias=1024.0 * EPS, scale=1.0), r=[ssk], w=["rstd2"])
                P.op("act", "activation", dict(out=rstd2, in_=rstd2, func=AF.Exp, scale=-0.5), r=["rstd2"], w=["rstd2"])

            for g in range(4):
                P.dma("sp", x1, xTv[:, :, gs(g)], w=[("x1", f) for f in range(8)])
                for f in range(8):
                    if f % 4 == 0:
                        wob, wobk = load_w("WO%d" % (f // 4))
                    pm, pmk = bank("mm", [0, 1])
                    for cc in range(8):
                        P.op("pe", "matmul", dict(out=pm[:], lhsT=wob[:, cc, (f % 4) * 128:(f % 4 + 1) * 128], rhs=merged[cc][:, gs(g)],
                                                   start=(cc == 0), stop=(cc == 7)), r=[wobk, ("merged", cc, g)], w=[pmk], inc=(cc == 7))
                    P.op("dve", "scalar_tensor_tensor", dict(out=x1[:, f, :], in0=pm[:], scalar=g1[:, f:f + 1], in1=x1[:, f, :],
                                                              op0=ALU.mult, op1=ALU.add), r=[pmk, ("x1", f), "adaT"], w=[("x1", f)])
                if dbg and b == 0 and g == 0:
                    P.dma("sp", dbgs["d_x1"].rearrange("(f p) t -> p f t", p=128), x1, r=[("x1", f) for f in range(8)])
                rms_stats([("x1", f) for f in range(8)])
                for f in range(8):
                    P.op("dve", "scalar_tensor_tensor", dict(out=tmp2[f % 2], in0=x1[:, f, :], scalar=a2[:, f:f + 1], in1=rstd2,
                                                              op0=ALU.mult, op1=ALU.mult), r=[("x1", f), "rstd2", "modv"], w=[("tmp2", f % 2)])
                    P.op("act", "activation", dict(out=h2[:, f, :], in_=tmp2[f % 2], func=AF.Identity, bias=sh2[:, f:f + 1], scale=1.0),
                         r=[("tmp2", f % 2), "adaT"], w=[("h2", f)])
                for ffc in range(32):
                    if ffc % 4 == 0:
                        f1b, f1k = load_w("F1_%d" % (ffc // 4))
                    pu, puk = bank("mm", [0, 1])
                    for k in range(8):
                        P.op("pe", "matmul", dict(out=pu[:], lhsT=f1b[:, k, (ffc % 4) * 128:(ffc % 4 + 1) * 128], rhs=h2[:, k, :],
                                                   start=(k == 0), stop=(k == 7)), r=[f1k, ("h2", k)], w=[puk], inc=(k == 7))
                    P.op("act", "activation", dict(out=rl[ffc % 2], in_=pu[:], func=AF.Relu), r=[puk], w=[("rl", ffc % 2)])
                    P.op("dve", "tensor_tensor", dict(out=u[:, ffc, :], in0=rl[ffc % 2], in1=rl[ffc % 2], op=ALU.mult),
                         r=[("rl", ffc % 2)], w=[("u", ffc)])
                for f in range(8):
                    f2b, f2k = load_w("F2_%d" % f)
                    pf, pfk = bank("S", [2, 3])
                    for ffc in range(32):
                        P.op("pe", "matmul", dict(out=pf[:], lhsT=f2b[:, ffc, :], rhs=u[:, ffc, :], start=(ffc == 0), stop=(ffc == 31)),
                             r=[f2k, ("u", ffc)], w=[pfk], inc=(ffc == 31))
                    P.op("dve", "scalar_tensor_tensor", dict(out=x1[:, f, :], in0=pf[:], scalar=g2[:, f:f + 1], in1=x1[:, f, :],
                                                              op0=ALU.mult, op1=ALU.add), r=[pfk, ("x1", f), "adaT"], w=[("x1", f)])
                rms_stats([("x1", f) for f in range(8)])
                for f in range(8):
                    P.op("dve", "scalar_tensor_tensor", dict(out=x1[:, f, :], in0=x1[:, f, :], scalar=fn32[:, f:f + 1], in1=rstd2,
                                                              op0=ALU.mult, op1=ALU.mult), r=[("x1", f), "rstd2", "fn32"], w=[("x1", f)])
                P.dma("pool", oTv[:, :, gs(g)], x1, r=[("x1", f) for f in range(8)], w=[("out", b, g)])
            P.barrier()
            chk()

        except _Stop:
            for e in P.CENG:
                for pt in P.pending[e]:
                    pt.val = P.cnt[e] + 1
            P.barrier_soft = True

        P.wait_all("sp", P.bar_toks)
        for e in ("sp", "pool"):
            for slot in P.dsem[e]:
                if slot[2] is not None:
                    P.wait_all("sp", [slot[2]])
        P.emit()
    return nc


def structural_constants():
    p = np.arange(128)
    cb = np.zeros((128, 2816), np.float32)
    cb[p, p] = 1.0
    sw = (p // 64) * 64 + ((p % 64) + 32) % 64
    cb[p, 128 + sw] = 1.0
    cc = np.arange(256)
    cb[:, 256:512] = np.where(cc[None, :] >= p[:, None], 0.0, -BIG)
    cb[:, 512:768] = np.where(cc[None, :] - 128 >= p[:, None], 0.0, -BIG)
    for r in range(16):
        cb[r, 768 + r * 128:768 + (r + 1) * 128] = 1.0
    cf = np.zeros((128, 904), np.float32)
    cf[:, 0:128] = (p[:, None] <= p[None, :]).astype(np.float32)
    t = np.arange(16)[:, None, None]
    j = np.arange(8)[None, None, :]
    past = np.broadcast_to(j < (t // 2), (16, 2, 8))
    own = np.broadcast_to(j == (t // 2), (16, 2, 8))
    cf[:, 128:384] = np.where(past, 0.0, -1e30).reshape(1, 256)
    cf[:, 384:640] = past.astype(np.float32).reshape(1, 256)
    cf[:, 640:896] = own.astype(np.float32).reshape(1, 256)
    cf[:, 896] = p % 32
    cf[:, 897] = np.where((p % 64) < 32, -1.0, 1.0)
    for c in range(4):
        cf[:, 898 + c] = 2 * c + p // 64
    pos = np.broadcast_to(np.arange(S, dtype=np.float32)[None, :], (128, S)).copy()
    return cb, cf, pos


_CACHE = {}


def make_in_maps(inputs, nbl, cores):
    f32 = np.float32
    x = np.asarray(inputs["x"], f32)
    c = np.asarray(inputs["c"], f32)
    cb, cf, pos = structural_constants()
    vecs = np.concatenate([np.asarray(inputs[k], f32).reshape(8, 128).T for k in ("ln1_w", "ln2_w", "final_norm_w", "ret_gn_w")], axis=1)
    shared = {
        "w_ada": np.ascontiguousarray(np.asarray(inputs["w_ada"], f32)[0]),
        "badaT": np.ascontiguousarray(np.asarray(inputs["b_ada"], f32)[0].reshape(48, 128).T),
        "vecs": np.ascontiguousarray(vecs),
        "w_in": np.ascontiguousarray(np.asarray(inputs["w_in"], f32)[0]),
        "w_moba_o": np.ascontiguousarray(np.asarray(inputs["w_moba_o"], f32)[0]),
        "w_ret_o": np.ascontiguousarray(np.asarray(inputs["w_ret_o"], f32)[0]),
        "w_out": np.ascontiguousarray(np.asarray(inputs["w_out"], f32)[0]),
        "w_ff1": np.ascontiguousarray(np.asarray(inputs["w_ff1"], f32)[0]),
        "w_ff2": np.ascontiguousarray(np.asarray(inputs["w_ff2"], f32)[0]),
        "cstb": cb, "cstf": cf, "posr": pos,
    }
    maps = []
    for i in range(cores):
        xb = x[i * nbl:(i + 1) * nbl]
        m = dict(shared)
        m["xT"] = np.ascontiguousarray(xb.transpose(0, 2, 1))
        cbt = c[i * nbl:(i + 1) * nbl]
        m["cT"] = np.ascontiguousarray(cbt.T.reshape(8, 128, nbl).transpose(1, 0, 2))
        maps.append(m)
    return maps


def kernel(**inputs):
    B = inputs["x"].shape[0]
    nbl = B // NCORES
    if nbl not in _CACHE:
        _CACHE[nbl] = build(nbl)
    nc = _CACHE[nbl]
    maps = make_in_maps(inputs, nbl, NCORES)
    res = run_bass_kernel_spmd(nc, maps, core_ids=list(range(NCORES)))
    outs = [np.asarray(r["outT"]).transpose(0, 2, 1) for r in res.results]
    return np.ascontiguousarray(np.concatenate(outs, axis=0).astype(np.float32))
```

```python
import math
import numpy as np
import concourse.bass as bass
import concourse.mybir as mybir
from concourse.bass_utils import run_bass_kernel_spmd
from contextlib import ExitStack

F32 = mybir.dt.float32
BF16 = mybir.dt.bfloat16
AF = mybir.ActivationFunctionType
ALU = mybir.AluOpType
AX = mybir.AxisListType

S = 2048
D = 1024
NCORES = 8
EPS = 1e-6
BIG = 30000.0
LN1E4 = math.log(10000.0)


class Tok:
    __slots__ = ("sem", "val", "eng")

    def __init__(self, sem, val, eng):
        self.sem = sem
        self.val = val
        self.eng = eng


class Prog:
    ENG = ["pe", "act", "dve", "pool", "sp"]
    CENG = ["pe", "act", "dve", "pool"]

    def __init__(self, nc, es):
        self.nc = nc
        self.q = {e: [] for e in self.ENG}
        self.sem = {e: es.enter_context(nc.semaphore("s_" + e)) for e in self.ENG}
        self.cnt = {e: 0 for e in self.ENG}
        self.last = {e: None for e in self.ENG}
        self.waited = {e: {} for e in self.ENG}
        self.res = {}
        self.pending = {e: [] for e in self.ENG}
        self.dsem = {}
        for e, n in (("sp", 28), ("pool", 12)):
            self.dsem[e] = [[es.enter_context(nc.semaphore("d_%s_%d" % (e, i))), 0, None] for i in range(n)]
        self.dnext = {e: 0 for e in self.dsem}
        self.dma_since_bar = []
        self.bar_toks = []
        self.nins = 0

    def _emit_wait(self, eng, tok):
        if tok is None:
            return
        assert tok.val is not None, "dependency on op whose completion inc is still pending"
        key = id(tok.sem)
        if self.waited[eng].get(key, 0) >= tok.val:
            return
        self.waited[eng][key] = tok.val
        self.q[eng].append(("wait_ge", (tok.sem, tok.val), {}, None))

    def _deps(self, eng, reads, writes):
        deps = []
        for k in reads:
            r = self.res.get(k)
            if r and r["w"] is not None:
                if not (r["w"].eng == eng and eng == "pe"):
                    deps.append(r["w"])
            if r and (k == "tpb" or (isinstance(k, tuple) and k[0] == "bank")):
                for e2, t in r["r"].items():
                    if e2 != eng:
                        deps.append(t)
        for k in writes:
            r = self.res.get(k)
            if r:
                w = r["w"]
                if w is not None and not (w.eng == eng and eng == "pe"):
                    deps.append(w)
                for e2, t in r["r"].items():
                    if e2 == eng and eng == "pe":
                        continue
                    deps.append(t)
                deps.extend(r["rd"])
        return deps

    def _record(self, tok, reads, writes, is_dma=False):
        for k in writes:
            self.res[k] = {"w": tok, "r": {}, "rd": []}
        for k in reads:
            r = self.res.setdefault(k, {"w": None, "r": {}, "rd": []})
            if is_dma:
                r["rd"].append(tok)
            else:
                r["r"][tok.eng] = tok

    def op(self, eng, name, kw, r=(), w=(), inc=True):
        for t in self._deps(eng, r, w):
            self._emit_wait(eng, t)
        self.nins += 1
        if inc:
            self.cnt[eng] += 1
            val = self.cnt[eng]
            tok = Tok(self.sem[eng], val, eng)
            self.q[eng].append((name, (), kw, (self.sem[eng], 1)))
            for pt in self.pending[eng]:
                pt.val = val
            self.pending[eng] = []
            self.last[eng] = tok
        else:
            self.q[eng].append((name, (), kw, None))
            tok = Tok(self.sem[eng], None, eng)
            self.pending[eng].append(tok)
        self._record(tok, r, w)
        return tok

    def dma(self, qeng, out, in_, r=(), w=(), after_bar=True):
        deng = "dma_" + qeng
        for t in self._deps(deng, r, w):
            self._emit_wait(qeng, t)
        if after_bar:
            for t in self.bar_toks:
                self._emit_wait(qeng, t)
        slot = self.dsem[qeng][self.dnext[qeng]]
        self.dnext[qeng] = (self.dnext[qeng] + 1) % len(self.dsem[qeng])
        if slot[2] is not None:
            self._emit_wait(qeng, slot[2])
        slot[1] += 16
        tok = Tok(slot[0], slot[1], deng)
        slot[2] = tok
        self.nins += 1
        self.q[qeng].append(("dma_start", (), dict(out=out, in_=in_), (slot[0], 16)))
        self._record(tok, r, w, is_dma=True)
        self.dma_since_bar.append(tok)
        return tok

    def barrier(self):
        for e in self.CENG:
            assert not self.pending[e], "pending incs at barrier on " + e
        toks = [self.last[e] for e in self.CENG if self.last[e] is not None]
        for e in self.CENG:
            for t in toks:
                if t.eng != e:
                    self._emit_wait(e, t)
            for t in self.dma_since_bar:
                self._emit_wait(e, t)
        self.bar_toks = toks + list(self.dma_since_bar)
        self.dma_since_bar = []

    def wait_all(self, eng, toks):
        for t in toks:
            self._emit_wait(eng, t)

    def emit(self):
        nc = self.nc

        def run(e, lst):
            for name, args, kw, inc in lst:
                ins = getattr(e, name)(*args, **kw)
                if inc is not None:
                    ins.then_inc(inc[0], inc[1])

        with nc.Block() as block:

            @block.tensor
            def _(e):
                run(e, self.q["pe"])

            @block.scalar
            def _(e):
                run(e, self.q["act"])

            @block.vector
            def _(e):
                run(e, self.q["dve"])

            @block.gpsimd
            def _(e):
                run(e, self.q["pool"])

            @block.sync
            def _(e):
                run(e, self.q["sp"])


def w_in_blocks():
    blks = {}
    for c in range(4):
        blks["M%d" % c] = [(c * 128, 128), (512 + c * 128, 128), (1024 + c * 128, 128)]
        blks["R%d" % c] = [(1536 + c * 128, 128), (2048 + c * 128, 128), (2560 + c * 256, 256)]
        blks["G%d" % c] = [(3584 + c * 256, 256)]
    for i in range(2):
        blks["GA%d" % i] = [(4608 + i * 512, 512)]
        blks["GR%d" % i] = [(5632 + i * 512, 512)]
    return blks


class _Stop(Exception):
    pass


def build(NBL, dbg=False, stop_after=None):
    nc = bass.Bass("TRN2", target_bir_lowering=False)

    def din(name, shape, dt=F32):
        return nc.dram_tensor(name, shape, dt, kind="ExternalInput").ap()

    xT = din("xT", [NBL, D, S])
    cT = din("cT", [128, 8, NBL])
    w_ada = din("w_ada", [D, 6 * D])
    badaT = din("badaT", [128, 48])
    vecs = din("vecs", [128, 32])
    w_in = din("w_in", [D, 6656])
    w_moba_o = din("w_moba_o", [512, D])
    w_ret_o = din("w_ret_o", [D, D])
    w_out = din("w_out", [D, D])
    w_ff1 = din("w_ff1", [D, 4 * D])
    w_ff2 = din("w_ff2", [4 * D, D])
    cstb = din("cstb", [128, 2816])
    cstf = din("cstf", [128, 904])
    posr = din("posr", [128, S])
    outT = nc.dram_tensor("outT", [NBL, D, S], F32, kind="ExternalOutput").ap()
    dbgs = {}
    if dbg:
        for nm, shp in (("d_h", [D, S]), ("d_q", [128, S]), ("d_k", [128, S]), ("d_a", [512, S]), ("d_g", [D, S]),
                        ("d_m", [D, S]), ("d_rq", [128, S])):
            dbgs[nm] = nc.dram_tensor(nm, shp, BF16, kind="ExternalOutput").ap()
        dbgs["d_ada"] = nc.dram_tensor("d_ada", [128, 48 * NBL], F32, kind="ExternalOutput").ap()
        dbgs["d_x1"] = nc.dram_tensor("d_x1", [D, 512], F32, kind="ExternalOutput").ap()

    def dscr(name, shape, dt=BF16):
        return nc.dram_tensor(name, shape, dt, kind="Internal").ap()

    inblks = w_in_blocks()
    wscr = {}
    for nm, pieces in inblks.items():
        nb = sum(p[1] for p in pieces)
        wscr[nm] = dscr("wb_" + nm, [128, 8, nb])
    for i in range(2):
        wscr["MO%d" % i] = dscr("wb_MO%d" % i, [128, 4, 512])
    for i in range(2):
        wscr["RO%d" % i] = dscr("wb_RO%d" % i, [128, 8, 512])
        wscr["WO%d" % i] = dscr("wb_WO%d" % i, [128, 8, 512])
    for i in range(8):
        wscr["F1_%d" % i] = dscr("wb_F1_%d" % i, [128, 8, 512])
        wscr["F2_%d" % i] = dscr("wb_F2_%d" % i, [128, 32, 128])
    tabs = dscr("tabs", [4, 128, S], F32)

    with ExitStack() as es:
        P = Prog(nc, es)

        def sb(name, shape, dt):
            return es.enter_context(nc.sbuf_tensor(name, shape, dt))

        def ps(name, shape, dt):
            return es.enter_context(nc.psum_tensor(name, shape, dt))

        cb = sb("cb", [128, 2816], BF16)
        identb = cb[:, 0:128]
        pswb = cb[:, 128:256]
        B01b = cb[:, 256:768]
        Eselb = cb[:, 768:2816]
        cf = sb("cf", [128, 904], F32)
        tri = cf[:, 0:128]
        pastbias = cf[:, 128:384]
        past01 = cf[:, 384:640]
        own01 = cf[:, 640:896]
        idxc = cf[:, 896:904]
        onesb = sb("onesb", [128, 128], BF16)
        o128b = sb("o128b", [128, 128], BF16)
        cmatb = sb("cmatb", [128, 128], BF16)
        vec = sb("vec", [128, 32], F32)
        bada = sb("bada", [128, 48], F32)
        adaT = sb("adaT", [128, 48, NBL], F32)
        modv = sb("modv", [128, NBL, 16], F32)
        fn32 = sb("fn32", [128, 8], F32)
        smallc = sb("smallc", [128, 24], F32)
        cact = sb("cact", [128, 8, NBL], F32)
        wring = sb("wring", [128, 4, 4096], BF16)
        regA = sb("regA", [128, 16384], BF16)
        regB = sb("regB", [128, 24576], BF16)
        regC = sb("regC", [128, 32768], BF16)

        banks = [ps("pb%d" % i, [128, 512], F32) for i in range(7)]
        tpb = ps("tpb", [128, 1024], BF16)
        brot = {}

        def bank(role, ids):
            i = brot.get(role, 0)
            brot[role] = i + 1
            b = ids[i % len(ids)]
            return banks[b], ("bank", b)

        wr = {"i": 0}

        def load_w(nm):
            slot = wr["i"] % 4
            wr["i"] += 1
            src = wscr[nm]
            kc, nb = src.shape[1], src.shape[2]
            dst = wring[:, slot, 0:kc * nb].rearrange("p (k n) -> p k n", n=nb)
            P.dma("sp", dst, src, r=[("wscr", nm)], w=[("wring", slot)], after_bar=False)
            return dst, ("wring", slot)

        P.dma("pool", cb[:], cstb, w=["cb"])
        P.dma("sp", cf[:], cstf, w=["cf"])
        P.dma("sp", vec[:], vecs, w=["vec"])
        P.dma("sp", bada[:], badaT, w=["bada"])
        P.dma("sp", cact[:], cT, w=["cact"])

        def conv(nm, wsrc, pieces):
            srcv = wsrc.rearrange("(k p) n -> p k n", p=128)
            off = 0
            nk = srcv.shape[1]
            for (c0, n) in pieces:
                for k0 in range(0, nk, 8):
                    P.dma("pool", wscr[nm][:, k0:min(k0 + 8, nk), off:off + n], srcv[:, k0:min(k0 + 8, nk), c0:c0 + n], w=[("wscrp", nm, off, k0)])
                off += n
            P.res[("wscr", nm)] = {"w": None, "r": {}, "rd": []}
            P._wscr_parts = getattr(P, "_wscr_parts", {})
            P._wscr_parts[nm] = [("wscrp", nm, o, k0) for o in np.cumsum([0] + [p[1] for p in pieces[:-1]]).tolist()
                                 for k0 in range(0, nk, 8)]

        conv_order = []
        for c in range(4):
            conv_order.append(("M%d" % c, w_in, inblks["M%d" % c]))
        for c in range(4):
            conv_order.append(("R%d" % c, w_in, inblks["R%d" % c]))
            conv_order.append(("G%d" % c, w_in, inblks["G%d" % c]))
        for i in range(2):
            conv_order.append(("MO%d" % i, w_moba_o, [(i * 512, 512)]))
            conv_order.append(("GA%d" % i, w_in, inblks["GA%d" % i]))
        for i in range(2):
            conv_order.append(("RO%d" % i, w_ret_o, [(i * 512, 512)]))
            conv_order.append(("GR%d" % i, w_in, inblks["GR%d" % i]))
        for i in range(2):
            conv_order.append(("WO%d" % i, w_out, [(i * 512, 512)]))
        for i in range(8):
            conv_order.append(("F1_%d" % i, w_ff1, [(i * 512, 512)]))
        for i in range(8):
            conv_order.append(("F2_%d" % i, w_ff2, [(i * 128, 128)]))
        for nm, wsrc, pieces in conv_order:
            conv(nm, wsrc, pieces)

        wseq = []
        for _b in range(NBL):
            wseq += ["M%d" % c for c in range(4)]
            for c in range(4):
                wseq += ["R%d" % c, "G%d" % c]
            wseq += ["MO0", "GA0", "MO1", "GA1", "GR0", "RO0", "GR1", "RO1"]
            for _g in range(4):
                wseq += ["WO0", "WO1"] + ["F1_%d" % i for i in range(8)] + ["F2_%d" % i for i in range(8)]
        wstate = {"issued": 0, "used": 0, "views": {}}
        PF = 2

        def _issue_next():
            i = wstate["issued"]
            if i >= len(wseq):
                return
            nm = wseq[i]
            slot = i % 4
            src = wscr[nm]
            kc, nb = src.shape[1], src.shape[2]
            dst = wring[:, slot, 0:kc * nb].rearrange("p (k n) -> p k n", n=nb)
            P.dma("sp", dst, src, r=P._wscr_parts[nm], w=[("wring", slot)], after_bar=False)
            wstate["views"][i] = (dst, ("wring", slot))
            wstate["issued"] += 1

        def load_w(nm):
            i = wstate["used"]
            assert wseq[i] == nm, (wseq[i], nm)
            while wstate["issued"] <= min(i + PF, len(wseq) - 1):
                _issue_next()
            wstate["used"] += 1
            return wstate["views"].pop(i)

        P.op("pool", "memset", dict(ap=onesb[:], constant=1.0), w=["onesb"])
        P.op("pool", "memset", dict(ap=o128b[:], constant=1.0 / 128.0), w=["o128b"])
        P.op("dve", "tensor_scalar", dict(out=cmatb[:], in0=identb, scalar1=-1.0 / 128.0, scalar2=None, op0=ALU.add),
             r=["cb"], w=["cmatb"])
        P.op("act", "activation", dict(out=smallc[:, 0:1], in_=idxc[:, 0:1], func=AF.Exp, scale=-LN1E4 / 32.0),
             r=["cf"], w=["sc0"])
        P.op("act", "activation", dict(out=smallc[:, 1:2], in_=idxc[:, 0:1], func=AF.Exp, scale=-LN1E4 / 31.0),
             r=["cf"], w=["sc1"])
        P.op("act", "activation", dict(out=smallc[:, 14:18], in_=idxc[:, 2:6], func=AF.Exp, scale=-math.log(2.0),
                                        bias=-5.0 * math.log(2.0)), r=["cf"], w=["sc14"])
        P.op("act", "activation", dict(out=smallc[:, 2:6], in_=smallc[:, 14:18], func=AF.Ln, scale=-1.0, bias=1.0),
             r=["sc14"], w=["sc2"])
        P.op("dve", "tensor_scalar", dict(out=smallc[:, 6:10], in0=smallc[:, 2:6], scalar1=-1.0, scalar2=None, op0=ALU.mult),
             r=["sc2"], w=["sc6"])
        P.op("dve", "tensor_scalar", dict(out=smallc[:, 10:14], in0=smallc[:, 2:6], scalar1=-1.0, scalar2=math.log(0.125),
                                           op0=ALU.mult, op1=ALU.add), r=["sc2"], w=["sc10"])
        P.op("dve", "tensor_scalar", dict(out=fn32[:], in0=vec[:, 16:24], scalar1=32.0, scalar2=None, op0=ALU.mult),
             r=["vec"], w=["fn32"])

        P.op("act", "activation", dict(out=cact[:], in_=cact[:], func=AF.Silu), r=["cact"], w=["cact"])
        wst = [regC[:, 0:8192].bitcast(F32).rearrange("p (k n) -> p k n", n=512),
               regC[:, 8192:16384].bitcast(F32).rearrange("p (k n) -> p k n", n=512)]
        wadav = w_ada.rearrange("(k p) n -> p k n", p=128)
        ada_ps, ada_key = banks[0], ("bank", 0)
        ada_v = ada_ps[:, 0:48 * NBL].rearrange("p (f b) -> p f b", b=NBL)
        for blk in range(12):
            st = wst[blk % 2]
            P.dma("sp", st, wadav[:, :, blk * 512:(blk + 1) * 512], w=[("wst", blk % 2)])
            for fi in range(4):
                f = blk * 4 + fi
                for k in range(8):
                    P.op("pe", "matmul", dict(out=ada_v[:, f, :], lhsT=st[:, k, fi * 128:(fi + 1) * 128], rhs=cact[:, k, :],
                                               start=(k == 0), stop=(k == 7)),
                         r=[("wst", blk % 2), "cact"], w=[("adaps", f)], inc=(k == 7))
        P.op("dve", "tensor_tensor", dict(out=adaT[:], in0=ada_v, in1=bada[:].unsqueeze(2).to_broadcast([128, 48, NBL]),
                                           op=ALU.add), r=[("adaps", f) for f in range(48)] + ["bada"], w=["adaT"])
        for b in range(NBL):
            P.op("dve", "scalar_tensor_tensor", dict(out=modv[:, b, 0:8], in0=adaT[:, 8:16, b], scalar=1.0, in1=vec[:, 0:8],
                                                      op0=ALU.add, op1=ALU.mult), r=["adaT", "vec"], w=[("modv", b, 0)])
            P.op("dve", "scalar_tensor_tensor", dict(out=modv[:, b, 8:16], in0=adaT[:, 32:40, b], scalar=1.0, in1=vec[:, 8:16],
                                                      op0=ALU.add, op1=ALU.mult), r=["adaT", "vec"], w=[("modv", b, 1)])
        P.op("dve", "tensor_scalar", dict(out=modv[:], in0=modv[:], scalar1=32.0, scalar2=None, op0=ALU.mult),
             r=[("modv", b, i) for b in range(NBL) for i in range(2)], w=["modv"])
        if dbg:
            P.dma("sp", dbgs["d_ada"], adaT[:].rearrange("p f b -> p (f b)"), r=["adaT"])

        posS = regC[:, 16384:20480].bitcast(F32)
        tA = regC[:, 20480:24576].bitcast(F32)
        tB = regC[:, 24576:28672].bitcast(F32)
        P.dma("sp", posS, posr, w=["posS"])
        TWO_PI = 2.0 * math.pi
        MAGIC = 12582912.0
        tK = regC[:, 28672:32768].bitcast(F32)
        for mi in range(2):
            for ti, phase in ((0, math.pi / 2), (1, 0.0)):
                P.op("dve", "tensor_scalar", dict(out=tA, in0=posS, scalar1=smallc[:, mi:mi + 1], scalar2=phase,
                                                   op0=ALU.mult, op1=ALU.add), r=["posS", "sc0", "sc1"], w=["tA"])
                P.op("dve", "tensor_scalar", dict(out=tK, in0=tA, scalar1=1.0 / TWO_PI, scalar2=MAGIC,
                                                   op0=ALU.mult, op1=ALU.add), r=["tA"], w=["tK"])
                P.op("dve", "tensor_scalar", dict(out=tK, in0=tK, scalar1=-MAGIC, scalar2=None, op0=ALU.add), r=["tK"], w=["tK"])
                P.op("dve", "scalar_tensor_tensor", dict(out=tA, in0=tK, scalar=-TWO_PI, in1=tA, op0=ALU.mult, op1=ALU.add),
                     r=["tK", "tA"], w=["tA"])
                P.op("dve", "tensor_scalar", dict(out=tA, in0=tA, scalar1=3.14159, scalar2=-3.14159, op0=ALU.min, op1=ALU.max),
                     r=["tA"], w=["tA"])
                if ti == 0:
                    P.op("act", "activation", dict(out=tB, in_=tA, func=AF.Sin), r=["tA"], w=["tB"])
                else:
                    P.op("act", "activation", dict(out=tA, in_=tA, func=AF.Sin), r=["tA"], w=["tA"])
                    P.op("dve", "tensor_scalar", dict(out=tB, in0=tA, scalar1=idxc[:, 1:2], scalar2=None, op0=ALU.mult),
                         r=["tA", "cf"], w=["tB"])
                P.dma("sp", tabs[mi * 2 + ti], tB, r=["tB"], w=[("tabs", mi * 2 + ti)])
        P.barrier()
        stage = [0]

        def chk():
            stage[0] += 1
            if stop_after is not None and stage[0] >= stop_after:
                raise _Stop()

        hT = [regA[:, c * 2048:(c + 1) * 2048] for c in range(8)]
        aT = [regB[:, c * 2048:(c + 1) * 2048] for c in range(4)]
        gT = [regB[:, 8192 + h * 2048:8192 + (h + 1) * 2048] for h in range(8)]
        merged = [regC[:, f * 2048:(f + 1) * 2048] for f in range(8)]

        def gs(g):
            return slice(g * 512, (g + 1) * 512)

        def proj_fm(pbank, pkey, wblk, wkey, c0, g, rkeys):
            for k in range(8):
                P.op("pe", "matmul", dict(out=pbank[:], lhsT=wblk[:, k, c0:c0 + 128], rhs=hT[k][:, gs(g)],
                                           start=(k == 0), stop=(k == 7)),
                     r=[wkey] + rkeys, w=[pkey], inc=(k == 7))

        try:
          for b in range(NBL):
            chk()
            a1 = modv[:, b, 0:8]
            a2 = modv[:, b, 8:16]
            sh1 = adaT[:, 0:8, b]
            g1 = adaT[:, 16:24, b]
            sh2 = adaT[:, 24:32, b]
            g2 = adaT[:, 40:48, b]
            hkeys = [("hT", k, g) for k in range(8) for g in range(4)]

            xs = [regC[:, c * 4096:(c + 1) * 4096].bitcast(F32) for c in range(8)]
            sq = [regB[:, 0:2048], regB[:, 2048:4096]]
            rstd = regB[:, 4096:8192].bitcast(F32)
            tmpn = [regB[:, 8192:12288].bitcast(F32), regB[:, 12288:16384].bitcast(F32)]
            for c in range(8):
                P.dma("sp", xs[c], xT[b, c * 128:(c + 1) * 128, :], w=[("xs", c)])
            for c in range(8):
                P.op("act", "activation", dict(out=sq[c % 2], in_=xs[c], func=AF.Square), r=[("xs", c)], w=[("sq", c % 2)])
                for g in range(4):
                    P.op("pe", "matmul", dict(out=banks[g][:], lhsT=onesb[:], rhs=sq[c % 2][:, gs(g)], start=(c == 0), stop=(c == 7)),
                         r=[("sq", c % 2), "onesb"], w=[("bank", g)], inc=(g == 3))
            for g in range(4):
                P.op("act", "activation", dict(out=rstd[:, gs(g)], in_=banks[g][:], func=AF.Ln, bias=1024.0 * EPS, scale=1.0),
                     r=[("bank", g)], w=[("rstd", g)])
                P.op("act", "activation", dict(out=rstd[:, gs(g)], in_=rstd[:, gs(g)], func=AF.Exp, scale=-0.5),
                     r=[("rstd", g)], w=[("rstd", g)])
            for c in range(8):
                P.op("dve", "scalar_tensor_tensor", dict(out=tmpn[c % 2], in0=xs[c], scalar=a1[:, c:c + 1], in1=rstd,
                                                          op0=ALU.mult, op1=ALU.mult),
                     r=[("xs", c), "modv"] + [("rstd", g) for g in range(4)], w=[("tmpn", c % 2)])
                P.op("act", "activation", dict(out=hT[c], in_=tmpn[c % 2], func=AF.Identity, bias=sh1[:, c:c + 1], scale=1.0),
                     r=[("tmpn", c % 2), "adaT"], w=[("hT", c, g) for g in range(4)])
            if dbg and b == 0:
                for c in range(8):
                    P.dma("sp", dbgs["d_h"][c * 128:(c + 1) * 128, :], hT[c], r=[("hT", c, 0)])
            P.barrier()
            chk()

            cosm = regC[:, 0:4096].bitcast(F32)
            sinm = regC[:, 4096:8192].bitcast(F32)
            qT = regC[:, 8192:10240]
            kT = regC[:, 10240:12288]
            vaug = regC[:, 12288:16384].rearrange("p (t h e) -> p t h e", t=16, h=2)
            q_bf = regC[:, 16384:16896]
            t1 = regC[:, 16896:17920].bitcast(F32)
            t2 = regC[:, 17920:18944].bitcast(F32)
            gm = regC[:, 18944:19456].bitcast(F32)
            cmpb = regC[:, 19456:21504]
            cnt = regC[:, 21504:22016].bitcast(F32)
            sel = regC[:, 22016:22528].bitcast(F32)
            btok = regC[:, 22528:22784]
            biasT = regC[:, 22784:24832]
            pTr = [regC[:, 24832 + i * 512:24832 + (i + 1) * 512] for i in range(3)] + [regC[:, 27648:28160]]
            rs = regC[:, 26368:27392].bitcast(F32)
            km = regC[:, 27392:27408].bitcast(F32)
            kmb = regC[:, 27408:27416]
            P.dma("sp", cosm, tabs[0], r=[("tabs", 0)], w=["cosm"])
            P.dma("sp", sinm, tabs[1], r=[("tabs", 1)], w=["sinm"])
            P.op("dve", "memset", dict(ap=vaug[:, :, :, 64:128], constant=1.0), w=["vones"])
            P.op("dve", "memset", dict(ap=biasT, constant=0.0), w=[("biasT", g) for g in range(4)])

            def rotary(dst, dkey, wblk, wkey, c0, cosT, sinT, ckeys, decay=None):
                for g in range(4):
                    pq, pk_ = bank("mm", [0, 1])
                    proj_fm(pq, pk_, wblk, wkey, c0, g, hkeys)
                    P.op("act", "activation", dict(out=q_bf, in_=pq[:], func=AF.Copy), r=[pk_], w=["q_bf"])
                    psw, psk = bank("sw", [2, 3])
                    P.op("pe", "matmul", dict(out=psw[:], lhsT=pswb, rhs=q_bf, start=True, stop=True), r=["q_bf", "cb"], w=[psk])
                    P.op("dve", "tensor_tensor", dict(out=t1, in0=pq[:], in1=cosT[:, gs(g)], op=ALU.mult), r=[pk_] + ckeys, w=["t1"])
                    P.op("dve", "tensor_tensor", dict(out=t2, in0=psw[:], in1=sinT[:, gs(g)], op=ALU.mult), r=[psk] + ckeys, w=["t2"])
                    if decay is None:
                        P.op("dve", "tensor_tensor", dict(out=dst[:, gs(g)], in0=t1, in1=t2, op=ALU.add), r=["t1", "t2"], w=[(dkey, g)])
                    else:
                        decay(g, dst, dkey)

            for c in range(4):
                wblk, wkey = load_w("M%d" % c)
                chk()
                rotary(qT, "qT", wblk, wkey, 0, cosm, sinm, ["cosm", "sinm"])
                rotary(kT, "kT", wblk, wkey, 128, cosm, sinm, ["cosm", "sinm"])
                if dbg and b == 0 and c == 0:
                    P.dma("sp", dbgs["d_q"], qT, r=[("qT", g) for g in range(4)])
                    P.dma("sp", dbgs["d_k"], kT, r=[("kT", g) for g in range(4)])
                chk()
                for tq in range(4):
                    pv, pvk = bank("mm", [0, 1])
                    for tt in range(4):
                        t = tq * 4 + tt
                        for k in range(8):
                            P.op("pe", "matmul", dict(out=pv[:, tt * 128:(tt + 1) * 128], lhsT=hT[k][:, t * 128:(t + 1) * 128],
                                                       rhs=wblk[:, k, 256:384], start=(k == 0), stop=(k == 7)),
                                 r=[wkey] + hkeys, w=[pvk], inc=(k == 7 and tt == 3))
                    P.op("act", "activation", dict(out=vaug[:, tq * 4:(tq + 1) * 4, :, 0:64],
                                                    in_=pv[:].rearrange("p (t h e) -> p t h e", t=4, h=2), func=AF.Copy),
                         r=[pvk], w=[("vaug", tq)])
                chk()
                P.op("dve", "tensor_reduce", dict(out=km, in_=kT.rearrange("p (j t) -> p j t", t=256), axis=AX.X, op=ALU.add),
                     r=[("kT", g) for g in range(4)], w=["km"])
                P.op("dve", "tensor_scalar", dict(out=kmb, in0=km, scalar1=1.0 / 256.0, scalar2=None, op0=ALU.mult), r=["km"], w=["kmb"])
                chk()
                gpsl = [(banks[4], ("bank", 4)), (banks[5], ("bank", 5))]
                gmv = gm.rearrange("p (t h j) -> p t h j", t=16, h=2)
                pbv = pastbias.rearrange("p (t h j) -> p t h j", t=16, h=2)
                for hh in range(2):
                    gps, gk = gpsl[hh]
                    gv = gps[:, 0:128].rearrange("p (t j) -> p t j", j=8)
                    for t in range(16):
                        P.op("pe", "matmul", dict(out=gv[:, t, :], lhsT=qT[hh * 64:(hh + 1) * 64, t * 128:(t + 1) * 128],
                                                   rhs=kmb[hh * 64:(hh + 1) * 64, :], start=True, stop=True),
                             r=[("qT", t // 4), "kmb"], w=[gk], inc=(t == 15))
                    P.op("dve", "tensor_tensor", dict(out=gmv[:, :, hh, :], in0=gv, in1=pbv[:, :, hh, :], op=ALU.add),
                         r=[gk, "cf"], w=[("gm", hh)])
                chk()
                gm3 = gm.rearrange("p (g j) -> p g j", j=8)
                P.op("dve", "tensor_tensor", dict(out=cmpb.rearrange("p (g a c) -> p g a c", a=8, c=8),
                                                   in0=gm3.unsqueeze(2).to_broadcast([128, 32, 8, 8]),
                                                   in1=gm3.unsqueeze(3).to_broadcast([128, 32, 8, 8]), op=ALU.is_gt),
                     r=[("gm", 0), ("gm", 1)], w=["cmpb"])
                P.op("dve", "tensor_reduce", dict(out=cnt, in_=cmpb.rearrange("p (g c) -> p g c", c=8), axis=AX.X, op=ALU.add),
                     r=["cmpb"], w=["cnt"])
                P.op("dve", "scalar_tensor_tensor", dict(out=sel, in0=cnt, scalar=2.5, in1=past01, op0=ALU.is_lt, op1=ALU.mult),
                     r=["cnt", "cf"], w=["sel"])
                P.op("dve", "tensor_tensor", dict(out=sel, in0=sel, in1=own01, op=ALU.add), r=["sel", "cf"], w=["sel"])
                P.op("dve", "tensor_scalar", dict(out=btok, in0=sel, scalar1=-1.0, scalar2=BIG, op0=ALU.add, op1=ALU.mult),
                     r=["sel"], w=["btok"])
                chk()
                btv = btok.rearrange("p (t x) -> p t x", x=16)
                for g in range(4):
                    for tt in range(4):
                        P.op("pe", "transpose", dict(out=tpb[0:16, tt * 128:(tt + 1) * 128], in_=btv[:, g * 4 + tt, :], identity=identb),
                             r=["btok", "cb"], w=["tpb"], inc=(tt == 3))
                    P.op("act", "activation", dict(out=biasT[0:16, gs(g)], in_=tpb[0:16, 0:512], func=AF.Copy), r=["tpb"], w=[("biasT", g)])
                chk()
                tiles = []
                for hh in range(2):
                    for qg in range(4):
                        full = list(range(0, 4 * qg + 2))
                        part = [4 * qg + 2, 4 * qg + 3]
                        order = [full[0]] + part + full[1:]
                        for oi, kt in enumerate(order):
                            tiles.append((hh, qg, oi, kt, oi == 0, oi == len(order) - 1))
                LOOK = 2
                sb_ids = [2, 3, 0, 1]
                st = {}

                def s_stage(i):
                    hh, qg, oi, kt, first, last = tiles[i]
                    hs = slice(hh * 64, (hh + 1) * 64)
                    j = kt // 2
                    c0 = 256 if j == 2 * qg + 1 else 0
                    cols = slice(c0, 512)
                    qcols = slice(qg * 512 + c0, (qg + 1) * 512)
                    diag = (j >= 2 * qg)
                    bi = sb_ids[i % 4]
                    sps, spk = banks[bi], ("bank", bi)
                    P.op("pe", "matmul", dict(out=sps[:, cols], lhsT=kT[hs, kt * 128:(kt + 1) * 128], rhs=qT[hs, qcols],
                                               start=True, stop=False), r=[("kT", kt // 4), ("qT", qg)], w=[spk], inc=False)
                    P.op("pe", "matmul", dict(out=sps[:, cols], lhsT=Eselb[:, (hh * 8 + j) * 128:(hh * 8 + j + 1) * 128],
                                               rhs=biasT[:, qcols], start=False, stop=(not diag)),
                         r=[("biasT", qg), "cb"], w=[spk], inc=(not diag))
                    if diag:
                        oc = (j - 2 * qg) * 256
                        P.op("pe", "matmul", dict(out=sps[:, oc:oc + 256], lhsT=identb, rhs=B01b[:, (kt % 2) * 256:(kt % 2 + 1) * 256],
                                                   start=False, stop=True), r=["cb"], w=[spk])
                    st[i] = (sps, spk, cols)

                for i in range(min(LOOK, len(tiles))):
                    s_stage(i)
                acc = acck = None
                for i in range(len(tiles)):
                    hh, qg, oi, kt, first, last = tiles[i]
                    hs = slice(hh * 64, (hh + 1) * 64)
                    if i + LOOK < len(tiles):
                        s_stage(i + LOOK)
                    sps, spk, cols = st.pop(i)
                    if first:
                        acc, acck = bank("acc", [5, 6])
                    pT = pTr[i % 4]
                    pkey = ("pT", i % 4)
                    P.op("act", "activation", dict(out=pT[:, cols], in_=sps[:, cols], func=AF.Exp, scale=0.125), r=[spk], w=[pkey])
                    P.op("pe", "matmul", dict(out=acc[:, cols], lhsT=vaug[:, kt, hh, :], rhs=pT[:, cols], start=first, stop=last),
                         r=[pkey, ("vaug", kt // 4), "vones"], w=[acck])
                    if last:
                        P.op("dve", "reciprocal", dict(out=rs[0:64, :], in_=acc[64:128, :]), r=[acck], w=["rs"])
                        P.op("dve", "tensor_tensor", dict(out=aT[c][hs, gs(qg)], in0=acc[0:64, :], in1=rs[0:64, :], op=ALU.mult),
                             r=[acck, "rs"], w=[("aT", c, hh, qg)])
            if dbg and b == 0:
                for c in range(4):
                    P.dma("sp", dbgs["d_a"][c * 128:(c + 1) * 128, :], aT[c], r=[("aT", c, hh, qg) for hh in range(2) for qg in range(4)])
            P.barrier()
            chk()

            cosr = regC[:, 0:4096].bitcast(F32)
            sinr = regC[:, 4096:8192].bitcast(F32)
            posR = regC[:, 8192:12288].bitcast(F32)
            qd = regC[:, 12288:14336]
            kd = regC[:, 14336:16384]
            vtok = regC[:, 16384:20480].rearrange("p (t e) -> p t e", e=256)
            q_bf = regC[:, 20480:20992]
            t1 = regC[:, 20992:22016].bitcast(F32)
            t2 = regC[:, 22016:23040].bitcast(F32)
            dq = regC[:, 23040:24064].bitcast(F32)
            dk = regC[:, 24064:25088].bitcast(F32)
            ssum = regC[:, 25088:26112].bitcast(F32)
            Smr = [regC[:, 26112 + i * 128:26112 + (i + 1) * 128] for i in range(2)]
            ktokr = [regC[:, 26368 + i * 64:26368 + (i + 1) * 64] for i in range(2)]
            Rf = [regC[:, 26496 + i * 256:26496 + (i + 1) * 256].bitcast(F32) for i in range(2)]
            Rbf = regC[:, 27008:27136]
            obf = [regC[:, 27136 + i * 512:27136 + (i + 1) * 512] for i in range(2)]
            sqb = [regC[:, 28160 + i * 512:28160 + (i + 1) * 512] for i in range(2)]
            rstdn = regC[:, 29184:30208].bitcast(F32)
            yn = regC[:, 30208:31232].bitcast(F32)
            srg = regC[:, 31232:31744]
            P.dma("sp", cosr, tabs[2], r=[("tabs", 2)], w=["cosr"])
            P.dma("sp", sinr, tabs[3], r=[("tabs", 3)], w=["sinr"])
            P.dma("sp", posR, posr, w=["posR"])
            for c in range(4):
                wblk, wkey = load_w("R%d" % c)
                gblk, gkey = load_w("G%d" % c)

                def dec_q(g, dst, dkey, c=c):
                    P.op("act", "activation", dict(out=dq, in_=posR[:, gs(g)], func=AF.Exp, scale=smallc[:, 2 + c:3 + c],
                                                    bias=smallc[:, 2 + c:3 + c]), r=["posR", "sc2"], w=["dq"])
                    P.op("dve", "tensor_tensor", dict(out=ssum, in0=t1, in1=t2, op=ALU.add), r=["t1", "t2"], w=["ssum"])
                    P.op("dve", "tensor_tensor", dict(out=dst[:, gs(g)], in0=ssum, in1=dq, op=ALU.mult), r=["ssum", "dq"], w=[(dkey, g)])

                def dec_k(g, dst, dkey, c=c):
                    P.op("act", "activation", dict(out=dk, in_=posR[:, gs(g)], func=AF.Exp, scale=smallc[:, 6 + c:7 + c],
                                                    bias=smallc[:, 10 + c:11 + c]), r=["posR", "sc6", "sc10"], w=["dk"])
                    P.op("dve", "tensor_tensor", dict(out=ssum, in0=t1, in1=t2, op=ALU.add), r=["t1", "t2"], w=["ssum"])
                    P.op("dve", "tensor_tensor", dict(out=dst[:, gs(g)], in0=ssum, in1=dk, op=ALU.mult), r=["ssum", "dk"], w=[(dkey, g)])

                rotary(qd, "qd", wblk, wkey, 0, cosr, sinr, ["cosr", "sinr"], decay=dec_q)
                rotary(kd, "kd", wblk, wkey, 128, cosr, sinr, ["cosr", "sinr"], decay=dec_k)
                if dbg and b == 0 and c == 0:
                    P.dma("sp", dbgs["d_rq"], qd, r=[("qd", g) for g in range(4)])
                for t2_ in range(8):
                    pv, pvk = bank("mm", [0, 1])
                    for tt in range(2):
                        t = t2_ * 2 + tt
                        for k in range(8):
                            P.op("pe", "matmul", dict(out=pv[:, tt * 256:(tt + 1) * 256], lhsT=hT[k][:, t * 128:(t + 1) * 128],
                                                       rhs=wblk[:, k, 256:512], start=(k == 0), stop=(k == 7)),
                                 r=[wkey] + hkeys, w=[pvk], inc=(k == 7 and tt == 1))
                    P.op("act", "activation", dict(out=vtok[:, t2_ * 2:(t2_ + 1) * 2, :],
                                                    in_=pv[:].rearrange("p (t e) -> p t e", e=256), func=AF.Copy),
                         r=[pvk], w=[("vtok", t2_)])
                P.op("dve", "memset", dict(ap=Rf[0][0:64, :], constant=0.0), w=[("Rf", 0)])
                P.op("dve", "memset", dict(ap=Rf[1][0:64, :], constant=0.0), w=[("Rf", 1)])
                ops_ = {}
                for n in range(16):
                    g = n // 4
                    ncol = slice(n * 128, (n + 1) * 128)
                    for hh in range(2):
                        h = 2 * c + hh
                        hs = slice(hh * 64, (hh + 1) * 64)
                        if n % 4 == 0:
                            ops_[hh] = bank("o", [5, 6])
                        o_ps, ok = ops_[hh]
                        ocol = slice((n % 4) * 128, (n % 4 + 1) * 128)
                        sps, spk = bank("S", [2, 3])
                        P.op("pe", "matmul", dict(out=sps[:, 0:128], lhsT=kd[hs, ncol], rhs=qd[hs, ncol], start=True, stop=True),
                             r=[("kd", g), ("qd", g)], w=[spk])
                        Sm = Smr[(n * 2 + hh) % 2]
                        smk = ("Sm", (n * 2 + hh) % 2)
                        P.op("dve", "tensor_tensor", dict(out=Sm, in0=sps[:, 0:128], in1=tri, op=ALU.mult), r=[spk, "cf"], w=[smk])
                        P.op("pe", "matmul", dict(out=o_ps[:, ocol], lhsT=vtok[:, n, hh * 128:(hh + 1) * 128], rhs=Sm, start=True, stop=(n == 0)),
                             r=[smk, ("vtok", n // 2)], w=[ok], inc=(n == 0))
                        if n > 0:
                            P.op("pe", "matmul", dict(out=o_ps[:, ocol], lhsT=Rbf[hs, :], rhs=qd[hs, ncol], start=False, stop=True),
                                 r=[("Rbf", hh), ("qd", g)], w=[ok])
                        if n < 15:
                            P.op("pe", "transpose", dict(out=tpb[:, 0:64], in_=kd[hs, ncol], identity=identb[hs, hs]),
                                 r=[("kd", g), "cb"], w=["tpb"])
                            ktok = ktokr[hh]
                            P.op("act", "activation", dict(out=ktok, in_=tpb[:, 0:64], func=AF.Copy), r=["tpb"], w=[("ktok", hh)])
                            kvp, kvk = banks[4], ("bank", 4)
                            P.op("pe", "matmul", dict(out=kvp[0:64, 0:128], lhsT=ktok, rhs=vtok[:, n, hh * 128:(hh + 1) * 128], start=True, stop=True),
                                 r=[("ktok", hh), ("vtok", n // 2)], w=[kvk])
                            P.op("dve", "tensor_tensor", dict(out=Rf[hh][0:64, :], in0=kvp[0:64, 0:128], in1=Rf[hh][0:64, :], op=ALU.add),
                                 r=[kvk, ("Rf", hh)], w=[("Rf", hh)])
                            P.op("act", "activation", dict(out=Rbf[hs, :], in_=Rf[hh][0:64, :], func=AF.Copy), r=[("Rf", hh)], w=[("Rbf", hh)])
                        if n % 4 == 3:
                            ob = obf[hh]
                            P.op("act", "activation", dict(out=ob, in_=o_ps[:], func=AF.Copy), r=[ok], w=[("obf", hh)])
                            cen, cenk = bank("mm", [0, 1])
                            P.op("pe", "matmul", dict(out=cen[:], lhsT=cmatb[:], rhs=ob, start=True, stop=True), r=[("obf", hh), "cmatb"], w=[cenk])
                            sq_ = sqb[hh]
                            P.op("act", "activation", dict(out=sq_, in_=cen[:], func=AF.Square), r=[cenk], w=[("sqb", hh)])
                            var, vark = banks[4], ("bank", 4)
                            P.op("pe", "matmul", dict(out=var[:], lhsT=o128b[:], rhs=sq_, start=True, stop=True), r=[("sqb", hh), "o128b"], w=[vark])
                            P.op("act", "activation", dict(out=rstdn, in_=var[:], func=AF.Ln, bias=EPS, scale=1.0), r=[vark], w=["rstdn"])
                            P.op("act", "activation", dict(out=rstdn, in_=rstdn, func=AF.Exp, scale=-0.5), r=["rstdn"], w=["rstdn"])
                            P.op("dve", "tensor_tensor", dict(out=yn, in0=cen[:], in1=rstdn, op=ALU.mult), r=[cenk, "rstdn"], w=["yn"])
                            prg, prgk = bank("mm", [0, 1])
                            proj_fm(prg, prgk, gblk, gkey, hh * 128, g, hkeys)
                            P.op("act", "activation", dict(out=srg, in_=prg[:], func=AF.Silu), r=[prgk], w=["srg"])
                            P.op("dve", "scalar_tensor_tensor", dict(out=gT[h][:, gs(g)], in0=yn, scalar=vec[:, 24 + h:25 + h], in1=srg,
                                                                      op0=ALU.mult, op1=ALU.mult), r=["yn", "srg", "vec"], w=[("gT", h, g)])
            if dbg and b == 0:
                for h in range(8):
                    P.dma("sp", dbgs["d_g"][h * 128:(h + 1) * 128, :], gT[h], r=[("gT", h, g) for g in range(4)])
            P.barrier()
            chk()

            sgr = [regC[:, 16384 + i * 512:16384 + (i + 1) * 512] for i in range(2)]
            tmpo = [regC[:, 17408 + i * 1024:17408 + (i + 1) * 1024].bitcast(F32) for i in range(2)]
            it = 0
            for f in range(8):
                if f % 4 == 0:
                    wmo, wmok = load_w("MO%d" % (f // 4))
                    gab, gabk = load_w("GA%d" % (f // 4))
                for g in range(4):
                    pga, pgak = bank("mm", [0, 1])
                    proj_fm(pga, pgak, gab, gabk, (f % 4) * 128, g, hkeys)
                    sg = sgr[it % 2]
                    P.op("act", "activation", dict(out=sg, in_=pga[:], func=AF.Sigmoid), r=[pgak], w=[("sgr", it % 2)])
                    pya, pyak = bank("S", [2, 3])
                    for cc in range(4):
                        P.op("pe", "matmul", dict(out=pya[:], lhsT=wmo[:, cc, (f % 4) * 128:(f % 4 + 1) * 128], rhs=aT[cc][:, gs(g)],
                                                   start=(cc == 0), stop=(cc == 3)),
                             r=[wmok] + [("aT", cc, hh, g) for hh in range(2)], w=[pyak], inc=(cc == 3))
                    P.op("dve", "tensor_tensor", dict(out=merged[f][:, gs(g)], in0=pya[:], in1=sg, op=ALU.mult),
                         r=[pyak, ("sgr", it % 2)], w=[("merged", f, g)])
                    it += 1
            for f in range(8):
                if f % 4 == 0:
                    grb, grbk = load_w("GR%d" % (f // 4))
                    rob, robk = load_w("RO%d" % (f // 4))
                for g in range(4):
                    pga, pgak = bank("mm", [0, 1])
                    proj_fm(pga, pgak, grb, grbk, (f % 4) * 128, g, hkeys)
                    sg = sgr[it % 2]
                    P.op("act", "activation", dict(out=sg, in_=pga[:], func=AF.Sigmoid), r=[pgak], w=[("sgr", it % 2)])
                    pyr, pyrk = bank("S", [2, 3])
                    for h in range(8):
                        P.op("pe", "matmul", dict(out=pyr[:], lhsT=rob[:, h, (f % 4) * 128:(f % 4 + 1) * 128], rhs=gT[h][:, gs(g)],
                                                   start=(h == 0), stop=(h == 7)), r=[robk, ("gT", h, g)], w=[pyrk], inc=(h == 7))
                    tm = tmpo[it % 2]
                    P.op("dve", "tensor_tensor", dict(out=tm, in0=pyr[:], in1=sg, op=ALU.mult), r=[pyrk, ("sgr", it % 2)], w=[("tmpo", it % 2)])
                    P.op("dve", "tensor_tensor", dict(out=merged[f][:, gs(g)], in0=merged[f][:, gs(g)], in1=tm, op=ALU.add),
                         r=[("merged", f, g), ("tmpo", it % 2)], w=[("merged", f, g)])
                    it += 1
            if dbg and b == 0:
                for f in range(8):
                    P.dma("sp", dbgs["d_m"][f * 128:(f + 1) * 128, :], merged[f], r=[("merged", f, g) for g in range(4)])
            P.barrier()
            chk()

            x1 = regA[:, 0:8192].bitcast(F32).rearrange("p (f t) -> p f t", t=512)
            h2 = regA[:, 8192:12288].rearrange("p (f t) -> p f t", t=512)
            sq2 = [regA[:, 12288 + i * 512:12288 + (i + 1) * 512] for i in range(2)]
            rstd2 = regA[:, 13312:14336].bitcast(F32)
            tmp2 = [regA[:, 14336 + i * 1024:14336 + (i + 1) * 1024].bitcast(F32) for i in range(2)]
            u = regB[:, 0:16384].rearrange("p (f t) -> p f t", t=512)
            rl = [regB[:, 16384 + i * 512:16384 + (i + 1) * 512] for i in range(2)]
            xTv = xT[b].rearrange("(f p) t -> p f t", p=128)
            oTv = outT[b].rearrange("(f p) t -> p f t", p=128)

            def rms_stats(src_keys):
                ssp, ssk = banks[4], ("bank", 4)
                for f in range(8):
                    P.op("act", "activation", dict(out=sq2[f % 2], in_=x1[:, f, :], func=AF.Square), r=[src_keys[f]], w=[("sq2", f % 2)])
                    P.op("pe", "matmul", dict(out=ssp[:], lhsT=onesb[:], rhs=sq2[f % 2], start=(f == 0), stop=(f == 7)),
                         r=[("sq2", f % 2), "onesb"], w=[ssk])
                P.op("act", "activation", dict(out=rstd2, in_=ssp[:], func=AF.Ln, bias=1024.0 * EPS, scale=1.0), r=[ssk], w=["rstd2"])
                P.op("act", "activation", dict(out=rstd2, in_=rstd2, func=AF.Exp, scale=-0.5), r=["rstd2"], w=["rstd2"])

            for g in range(4):
                P.dma("sp", x1, xTv[:, :, gs(g)], w=[("x1", f) for f in range(8)])
                for f in range(8):
                    if f % 4 == 0:
                        wob, wobk = load_w("WO%d" % (f // 4))
                    pm, pmk = bank("mm", [0, 1])
                    for cc in range(8):
                        P.op("pe", "matmul", dict(out=pm[:], lhsT=wob[:, cc, (f % 4) * 128:(f % 4 + 1) * 128], rhs=merged[cc][:, gs(g)],
                                                   start=(cc == 0), stop=(cc == 7)), r=[wobk, ("merged", cc, g)], w=[pmk], inc=(cc == 7))
                    P.op("dve", "scalar_tensor_tensor", dict(out=x1[:, f, :], in0=pm[:], scalar=g1[:, f:f + 1], in1=x1[:, f, :],
                                                              op0=ALU.mult, op1=ALU.add), r=[pmk, ("x1", f), "adaT"], w=[("x1", f)])
                if dbg and b == 0 and g == 0:
                    P.dma("sp", dbgs["d_x1"].rearrange("(f p) t -> p f t", p=128), x1, r=[("x1", f) for f in range(8)])
                rms_stats([("x1", f) for f in range(8)])
                for f in range(8):
                    P.op("dve", "scalar_tensor_tensor", dict(out=tmp2[f % 2], in0=x1[:, f, :], scalar=a2[:, f:f + 1], in1=rstd2,
                                                              op0=ALU.mult, op1=ALU.mult), r=[("x1", f), "rstd2", "modv"], w=[("tmp2", f % 2)])
                    P.op("act", "activation", dict(out=h2[:, f, :], in_=tmp2[f % 2], func=AF.Identity, bias=sh2[:, f:f + 1], scale=1.0),
                         r=[("tmp2", f % 2), "adaT"], w=[("h2", f)])
                for ffc in range(32):
                    if ffc % 4 == 0:
                        f1b, f1k = load_w("F1_%d" % (ffc // 4))
                    pu, puk = bank("mm", [0, 1])
                    for k in range(8):
                        P.op("pe", "matmul", dict(out=pu[:], lhsT=f1b[:, k, (ffc % 4) * 128:(ffc % 4 + 1) * 128], rhs=h2[:, k, :],
                                                   start=(k == 0), stop=(k == 7)), r=[f1k, ("h2", k)], w=[puk], inc=(k == 7))
                    P.op("act", "activation", dict(out=rl[ffc % 2], in_=pu[:], func=AF.Relu), r=[puk], w=[("rl", ffc % 2)])
                    P.op("dve", "tensor_tensor", dict(out=u[:, ffc, :], in0=rl[ffc % 2], in1=rl[ffc % 2], op=ALU.mult),
                         r=[("rl", ffc % 2)], w=[("u", ffc)])
                for f in range(8):
                    f2b, f2k = load_w("F2_%d" % f)
                    pf, pfk = bank("S", [2, 3])
                    for ffc in range(32):
                        P.op("pe", "matmul", dict(out=pf[:], lhsT=f2b[:, ffc, :], rhs=u[:, ffc, :], start=(ffc == 0), stop=(ffc == 31)),
                             r=[f2k, ("u", ffc)], w=[pfk], inc=(ffc == 31))
                    P.op("dve", "scalar_tensor_tensor", dict(out=x1[:, f, :], in0=pf[:], scalar=g2[:, f:f + 1], in1=x1[:, f, :],
                                                              op0=ALU.mult, op1=ALU.add), r=[pfk, ("x1", f), "adaT"], w=[("x1", f)])
                rms_stats([("x1", f) for f in range(8)])
                for f in range(8):
                    P.op("dve", "scalar_tensor_tensor", dict(out=x1[:, f, :], in0=x1[:, f, :], scalar=fn32[:, f:f + 1], in1=rstd2,
                                                              op0=ALU.mult, op1=ALU.mult), r=[("x1", f), "rstd2", "fn32"], w=[("x1", f)])
                P.dma("pool", oTv[:, :, gs(g)], x1, r=[("x1", f) for f in range(8)], w=[("out", b, g)])
            P.barrier()
            chk()

        except _Stop:
            for e in P.CENG:
                for pt in P.pending[e]:
                    pt.val = P.cnt[e] + 1
            P.barrier_soft = True

        P.wait_all("sp", P.bar_toks)
        for e in ("sp", "pool"):
            for slot in P.dsem[e]:
                if slot[2] is not None:
                    P.wait_all("sp", [slot[2]])
        P.emit()
    return nc


def structural_constants():
    p = np.arange(128)
    cb = np.zeros((128, 2816), np.float32)
    cb[p, p] = 1.0
    sw = (p // 64) * 64 + ((p % 64) + 32) % 64
    cb[p, 128 + sw] = 1.0
    cc = np.arange(256)
    cb[:, 256:512] = np.where(cc[None, :] >= p[:, None], 0.0, -BIG)
    cb[:, 512:768] = np.where(cc[None, :] - 128 >= p[:, None], 0.0, -BIG)
    for r in range(16):
        cb[r, 768 + r * 128:768 + (r + 1) * 128] = 1.0
    cf = np.zeros((128, 904), np.float32)
    cf[:, 0:128] = (p[:, None] <= p[None, :]).astype(np.float32)
    t = np.arange(16)[:, None, None]
    j = np.arange(8)[None, None, :]
    past = np.broadcast_to(j < (t // 2), (16, 2, 8))
    own = np.broadcast_to(j == (t // 2), (16, 2, 8))
    cf[:, 128:384] = np.where(past, 0.0, -1e30).reshape(1, 256)
    cf[:, 384:640] = past.astype(np.float32).reshape(1, 256)
    cf[:, 640:896] = own.astype(np.float32).reshape(1, 256)
    cf[:, 896] = p % 32
    cf[:, 897] = np.where((p % 64) < 32, -1.0, 1.0)
    for c in range(4):
        cf[:, 898 + c] = 2 * c + p // 64
    pos = np.broadcast_to(np.arange(S, dtype=np.float32)[None, :], (128, S)).copy()
    return cb, cf, pos


_CACHE = {}


def make_in_maps(inputs, nbl, cores):
    f32 = np.float32
    x = np.asarray(inputs["x"], f32)
    c = np.asarray(inputs["c"], f32)
    cb, cf, pos = structural_constants()
    vecs = np.concatenate([np.asarray(inputs[k], f32).reshape(8, 128).T for k in ("ln1_w", "ln2_w", "final_norm_w", "ret_gn_w")], axis=1)
    shared = {
        "w_ada": np.ascontiguousarray(np.asarray(inputs["w_ada"], f32)[0]),
        "badaT": np.ascontiguousarray(np.asarray(inputs["b_ada"], f32)[0].reshape(48, 128).T),
        "vecs": np.ascontiguousarray(vecs),
        "w_in": np.ascontiguousarray(np.asarray(inputs["w_in"], f32)[0]),
        "w_moba_o": np.ascontiguousarray(np.asarray(inputs["w_moba_o"], f32)[0]),
        "w_ret_o": np.ascontiguousarray(np.asarray(inputs["w_ret_o"], f32)[0]),
        "w_out": np.ascontiguousarray(np.asarray(inputs["w_out"], f32)[0]),
        "w_ff1": np.ascontiguousarray(np.asarray(inputs["w_ff1"], f32)[0]),
        "w_ff2": np.ascontiguousarray(np.asarray(inputs["w_ff2"], f32)[0]),
        "cstb": cb, "cstf": cf, "posr": pos,
    }
    maps = []
    for i in range(cores):
        xb = x[i * nbl:(i + 1) * nbl]
        m = dict(shared)
        m["xT"] = np.ascontiguousarray(xb.transpose(0, 2, 1))
        cbt = c[i * nbl:(i + 1) * nbl]
        m["cT"] = np.ascontiguousarray(cbt.T.reshape(8, 128, nbl).transpose(1, 0, 2))
        maps.append(m)
    return maps


def kernel(**inputs):
    B = inputs["x"].shape[0]
    nbl = B // NCORES
    if nbl not in _CACHE:
        _CACHE[nbl] = build(nbl)
    nc = _CACHE[nbl]
    maps = make_in_maps(inputs, nbl, NCORES)
    res = run_bass_kernel_spmd(nc, maps, core_ids=list(range(NCORES)))
    outs = [np.asarray(r["outT"]).transpose(0, 2, 1) for r in res.results]
    return np.ascontiguousarray(np.concatenate(outs, axis=0).astype(np.float32))
```

```python
import math
import numpy as np
import concourse.bass as bass
import concourse.mybir as mybir
from concourse.bass_utils import run_bass_kernel_spmd
from contextlib import ExitStack

F32 = mybir.dt.float32
BF16 = mybir.dt.bfloat16
AF = mybir.ActivationFunctionType
ALU = mybir.AluOpType
AX = mybir.AxisListType

S = 2048
D = 1024
NCORES = 8
EPS = 1e-6
BIG = 30000.0
LN1E4 = math.log(10000.0)


class Tok:
    __slots__ = ("sem", "val", "eng")

    def __init__(self, sem, val, eng):
        self.sem = sem
        self.val = val
        self.eng = eng


class Prog:
    ENG = ["pe", "act", "dve", "pool", "sp"]
    CENG = ["pe", "act", "dve", "pool"]

    def __init__(self, nc, es):
        self.nc = nc
        self.q = {e: [] for e in self.ENG}
        self.sem = {e: es.enter_context(nc.semaphore("s_" + e)) for e in self.ENG}
        self.cnt = {e: 0 for e in self.ENG}
        self.last = {e: None for e in self.ENG}
        self.waited = {e: {} for e in self.ENG}
        self.res = {}
        self.pending = {e: [] for e in self.ENG}
        self.dsem = {}
        for e, n in (("sp", 28), ("pool", 12)):
            self.dsem[e] = [[es.enter_context(nc.semaphore("d_%s_%d" % (e, i))), 0, None] for i in range(n)]
        self.dnext = {e: 0 for e in self.dsem}
        self.dma_since_bar = []
        self.bar_toks = []
        self.nins = 0

    def _emit_wait(self, eng, tok):
        if tok is None:
            return
        assert tok.val is not None, "dependency on op whose completion inc is still pending"
        key = id(tok.sem)
        if self.waited[eng].get(key, 0) >= tok.val:
            return
        self.waited[eng][key] = tok.val
        self.q[eng].append(("wait_ge", (tok.sem, tok.val), {}, None))

    def _deps(self, eng, reads, writes):
        deps = []
        for k in reads:
            r = self.res.get(k)
            if r and r["w"] is not None:
                if not (r["w"].eng == eng and eng == "pe"):
                    deps.append(r["w"])
            if r and (k == "tpb" or (isinstance(k, tuple) and k[0] == "bank")):
                for e2, t in r["r"].items():
                    if e2 != eng:
                        deps.append(t)
        for k in writes:
            r = self.res.get(k)
            if r:
                w = r["w"]
                if w is not None and not (w.eng == eng and eng == "pe"):
                    deps.append(w)
                for e2, t in r["r"].items():
                    if e2 == eng and eng == "pe":
                        continue
                    deps.append(t)
                deps.extend(r["rd"])
        return deps

    def _record(self, tok, reads, writes, is_dma=False):
        for k in writes:
            self.res[k] = {"w": tok, "r": {}, "rd": []}
        for k in reads:
            r = self.res.setdefault(k, {"w": None, "r": {}, "rd": []})
            if is_dma:
                r["rd"].append(tok)
            else:
                r["r"][tok.eng] = tok

    def op(self, eng, name, kw, r=(), w=(), inc=True):
        for t in self._deps(eng, r, w):
            self._emit_wait(eng, t)
        self.nins += 1
        if inc:
            self.cnt[eng] += 1
            val = self.cnt[eng]
            tok = Tok(self.sem[eng], val, eng)
            self.q[eng].append((name, (), kw, (self.sem[eng], 1)))
            for pt in self.pending[eng]:
                pt.val = val
            self.pending[eng] = []
            self.last[eng] = tok
        else:
            self.q[eng].append((name, (), kw, None))
            tok = Tok(self.sem[eng], None, eng)
            self.pending[eng].append(tok)
        self._record(tok, r, w)
        return tok

    def dma(self, qeng, out, in_, r=(), w=(), after_bar=True):
        deng = "dma_" + qeng
        for t in self._deps(deng, r, w):
            self._emit_wait(qeng, t)
        if after_bar:
            for t in self.bar_toks:
                self._emit_wait(qeng, t)
        slot = self.dsem[qeng][self.dnext[qeng]]
        self.dnext[qeng] = (self.dnext[qeng] + 1) % len(self.dsem[qeng])
        if slot[2] is not None:
            self._emit_wait(qeng, slot[2])
        slot[1] += 16
        tok = Tok(slot[0], slot[1], deng)
        slot[2] = tok
        self.nins += 1
        self.q[qeng].append(("dma_start", (), dict(out=out, in_=in_), (slot[0], 16)))
        self._record(tok, r, w, is_dma=True)
        self.dma_since_bar.append(tok)
        return tok

    def barrier(self):
        for e in self.CENG:
            assert not self.pending[e], "pending incs at barrier on " + e
        toks = [self.last[e] for e in self.CENG if self.last[e] is not None]
        for e in self.CENG:
            for t in toks:
                if t.eng != e:
                    self._emit_wait(e, t)
            for t in self.dma_since_bar:
                self._emit_wait(e, t)
        self.bar_toks = toks + list(self.dma_since_bar)
        self.dma_since_bar = []

    def wait_all(self, eng, toks):
        for t in toks:
            self._emit_wait(eng, t)

    def emit(self):
        nc = self.nc

        def run(e, lst):
            for name, args, kw, inc in lst:
                ins = getattr(e, name)(*args, **kw)
                if inc is not None:
                    ins.then_inc(inc[0], inc[1])

        with nc.Block() as block:

            @block.tensor
            def _(e):
                run(e, self.q["pe"])

            @block.scalar
            def _(e):
                run(e, self.q["act"])

            @block.vector
            def _(e):
                run(e, self.q["dve"])

            @block.gpsimd
            def _(e):
                run(e, self.q["pool"])

            @block.sync
            def _(e):
                run(e, self.q["sp"])


def w_in_blocks():
    blks = {}
    for c in range(4):
        blks["M%d" % c] = [(c * 128, 128), (512 + c * 128, 128), (1024 + c * 128, 128)]
        blks["R%d" % c] = [(1536 + c * 128, 128), (2048 + c * 128, 128), (2560 + c * 256, 256)]
        blks["G%d" % c] = [(3584 + c * 256, 256)]
    for i in range(2):
        blks["GA%d" % i] = [(4608 + i * 512, 512)]
        blks["GR%d" % i] = [(5632 + i * 512, 512)]
    return blks


class _Stop(Exception):
    pass


def build(NBL, dbg=False, stop_after=None):
    nc = bass.Bass("TRN2", target_bir_lowering=False)

    def din(name, shape, dt=F32):
        return nc.dram_tensor(name, shape, dt, kind="ExternalInput").ap()

    xT = din("xT", [NBL, D, S])
    cT = din("cT", [128, 8, NBL])
    w_ada = din("w_ada", [D, 6 * D])
    badaT = din("badaT", [128, 48])
    vecs = din("vecs", [128, 32])
    w_in = din("w_in", [D, 6656])
    w_moba_o = din("w_moba_o", [512, D])
    w_ret_o = din("w_ret_o", [D, D])
    w_out = din("w_out", [D, D])
    w_ff1 = din("w_ff1", [D, 4 * D])
    w_ff2 = din("w_ff2", [4 * D, D])
    cstb = din("cstb", [128, 2816])
    cstf = din("cstf", [128, 904])
    posr = din("posr", [128, S])
    outT = nc.dram_tensor("outT", [NBL, D, S], F32, kind="ExternalOutput").ap()
    dbgs = {}
    if dbg:
        for nm, shp in (("d_h", [D, S]), ("d_q", [128, S]), ("d_k", [128, S]), ("d_a", [512, S]), ("d_g", [D, S]),
                        ("d_m", [D, S]), ("d_rq", [128, S])):
            dbgs[nm] = nc.dram_tensor(nm, shp, BF16, kind="ExternalOutput").ap()
        dbgs["d_ada"] = nc.dram_tensor("d_ada", [128, 48 * NBL], F32, kind="ExternalOutput").ap()
        dbgs["d_x1"] = nc.dram_tensor("d_x1", [D, 512], F32, kind="ExternalOutput").ap()

    def dscr(name, shape, dt=BF16):
        return nc.dram_tensor(name, shape, dt, kind="Internal").ap()

    inblks = w_in_blocks()
    wscr = {}
    for nm, pieces in inblks.items():
        nb = sum(p[1] for p in pieces)
        wscr[nm] = dscr("wb_" + nm, [128, 8, nb])
    for i in range(2):
        wscr["MO%d" % i] = dscr("wb_MO%d" % i, [128, 4, 512])
    for i in range(2):
        wscr["RO%d" % i] = dscr("wb_RO%d" % i, [128, 8, 512])
        wscr["WO%d" % i] = dscr("wb_WO%d" % i, [128, 8, 512])
    for i in range(8):
        wscr["F1_%d" % i] = dscr("wb_F1_%d" % i, [128, 8, 512])
        wscr["F2_%d" % i] = dscr("wb_F2_%d" % i, [128, 32, 128])
    tabs = dscr("tabs", [4, 128, S], F32)

    with ExitStack() as es:
        P = Prog(nc, es)

        def sb(name, shape, dt):
            return es.enter_context(nc.sbuf_tensor(name, shape, dt))

        def ps(name, shape, dt):
            return es.enter_context(nc.psum_tensor(name, shape, dt))

        cb = sb("cb", [128, 2816], BF16)
        identb = cb[:, 0:128]
        pswb = cb[:, 128:256]
        B01b = cb[:, 256:768]
        Eselb = cb[:, 768:2816]
        cf = sb("cf", [128, 904], F32)
        tri = cf[:, 0:128]
        pastbias = cf[:, 128:384]
        past01 = cf[:, 384:640]
        own01 = cf[:, 640:896]
        idxc = cf[:, 896:904]
        onesb = sb("onesb", [128, 128], BF16)
        o128b = sb("o128b", [128, 128], BF16)
        cmatb = sb("cmatb", [128, 128], BF16)
        vec = sb("vec", [128, 32], F32)
        bada = sb("bada", [128, 48], F32)
        adaT = sb("adaT", [128, 48, NBL], F32)
        modv = sb("modv", [128, NBL, 16], F32)
        fn32 = sb("fn32", [128, 8], F32)
        smallc = sb("smallc", [128, 24], F32)
        cact = sb("cact", [128, 8, NBL], F32)
        wring = sb("wring", [128, 4, 4096], BF16)
        regA = sb("regA", [128, 16384], BF16)
        regB = sb("regB", [128, 24576], BF16)
        regC = sb("regC", [128, 32768], BF16)
        regD = sb("regD", [128, 4096], BF16)

        banks = [ps("pb%d" % i, [128, 512], F32) for i in range(7)]
        tpb = ps("tpb", [128, 1024], BF16)
        brot = {}

        def bank(role, ids):
            i = brot.get(role, 0)
            brot[role] = i + 1
            b = ids[i % len(ids)]
            return banks[b], ("bank", b)

        wr = {"i": 0}

        def load_w(nm):
            slot = wr["i"] % 4
            wr["i"] += 1
            src = wscr[nm]
            kc, nb = src.shape[1], src.shape[2]
            dst = wring[:, slot, 0:kc * nb].rearrange("p (k n) -> p k n", n=nb)
            P.dma("sp", dst, src, r=[("wscr", nm)], w=[("wring", slot)], after_bar=False)
            return dst, ("wring", slot)

        P.dma("pool", cb[:], cstb, w=["cb"])
        P.dma("sp", cf[:], cstf, w=["cf"])
        P.dma("sp", vec[:], vecs, w=["vec"])
        P.dma("sp", bada[:], badaT, w=["bada"])
        P.dma("sp", cact[:], cT, w=["cact"])

        def conv(nm, wsrc, pieces):
            srcv = wsrc.rearrange("(k p) n -> p k n", p=128)
            off = 0
            nk = srcv.shape[1]
            for (c0, n) in pieces:
                for k0 in range(0, nk, 8):
                    P.dma("pool", wscr[nm][:, k0:min(k0 + 8, nk), off:off + n], srcv[:, k0:min(k0 + 8, nk), c0:c0 + n], w=[("wscrp", nm, off, k0)])
                off += n
            P.res[("wscr", nm)] = {"w": None, "r": {}, "rd": []}
            P._wscr_parts = getattr(P, "_wscr_parts", {})
            P._wscr_parts[nm] = [("wscrp", nm, o, k0) for o in np.cumsum([0] + [p[1] for p in pieces[:-1]]).tolist()
                                 for k0 in range(0, nk, 8)]

        conv_order = []
        for c in range(4):
            conv_order.append(("M%d" % c, w_in, inblks["M%d" % c]))
        for c in range(4):
            conv_order.append(("R%d" % c, w_in, inblks["R%d" % c]))
            conv_order.append(("G%d" % c, w_in, inblks["G%d" % c]))
        for i in range(2):
            conv_order.append(("MO%d" % i, w_moba_o, [(i * 512, 512)]))
            conv_order.append(("GA%d" % i, w_in, inblks["GA%d" % i]))
        for i in range(2):
            conv_order.append(("RO%d" % i, w_ret_o, [(i * 512, 512)]))
            conv_order.append(("GR%d" % i, w_in, inblks["GR%d" % i]))
        for i in range(2):
            conv_order.append(("WO%d" % i, w_out, [(i * 512, 512)]))
        for i in range(8):
            conv_order.append(("F1_%d" % i, w_ff1, [(i * 512, 512)]))
        for i in range(8):
            conv_order.append(("F2_%d" % i, w_ff2, [(i * 128, 128)]))
        for nm, wsrc, pieces in conv_order:
            conv(nm, wsrc, pieces)

        wseq = []
        for _b in range(NBL):
            wseq += ["M%d" % c for c in range(4)]
            for c in range(4):
                wseq += ["R%d" % c, "G%d" % c]
            wseq += ["MO0", "GA0", "MO1", "GA1", "GR0", "RO0", "GR1", "RO1"]
            for _g in range(4):
                wseq += ["WO0", "WO1"] + ["F1_%d" % i for i in range(8)] + ["F2_%d" % i for i in range(8)]
        wstate = {"issued": 0, "used": 0, "views": {}}
        PF = 2

        def _issue_next():
            i = wstate["issued"]
            if i >= len(wseq):
                return
            nm = wseq[i]
            slot = i % 4
            src = wscr[nm]
            kc, nb = src.shape[1], src.shape[2]
            dst = wring[:, slot, 0:kc * nb].rearrange("p (k n) -> p k n", n=nb)
            P.dma("sp", dst, src, r=P._wscr_parts[nm], w=[("wring", slot)], after_bar=False)
            wstate["views"][i] = (dst, ("wring", slot))
            wstate["issued"] += 1

        def load_w(nm):
            i = wstate["used"]
            assert wseq[i] == nm, (wseq[i], nm)
            while wstate["issued"] <= min(i + PF, len(wseq) - 1):
                _issue_next()
            wstate["used"] += 1
            return wstate["views"].pop(i)

        P.op("pool", "memset", dict(ap=onesb[:], constant=1.0), w=["onesb"])
        P.op("pool", "memset", dict(ap=o128b[:], constant=1.0 / 128.0), w=["o128b"])
        P.op("dve", "tensor_scalar", dict(out=cmatb[:], in0=identb, scalar1=-1.0 / 128.0, scalar2=None, op0=ALU.add),
             r=["cb"], w=["cmatb"])
        P.op("act", "activation", dict(out=smallc[:, 0:1], in_=idxc[:, 0:1], func=AF.Exp, scale=-LN1E4 / 32.0),
             r=["cf"], w=["sc0"])
        P.op("act", "activation", dict(out=smallc[:, 1:2], in_=idxc[:, 0:1], func=AF.Exp, scale=-LN1E4 / 31.0),
             r=["cf"], w=["sc1"])
        P.op("act", "activation", dict(out=smallc[:, 14:18], in_=idxc[:, 2:6], func=AF.Exp, scale=-math.log(2.0),
                                        bias=-5.0 * math.log(2.0)), r=["cf"], w=["sc14"])
        P.op("act", "activation", dict(out=smallc[:, 2:6], in_=smallc[:, 14:18], func=AF.Ln, scale=-1.0, bias=1.0),
             r=["sc14"], w=["sc2"])
        P.op("dve", "tensor_scalar", dict(out=smallc[:, 6:10], in0=smallc[:, 2:6], scalar1=-1.0, scalar2=None, op0=ALU.mult),
             r=["sc2"], w=["sc6"])
        P.op("dve", "tensor_scalar", dict(out=smallc[:, 10:14], in0=smallc[:, 2:6], scalar1=-1.0, scalar2=math.log(0.125),
                                           op0=ALU.mult, op1=ALU.add), r=["sc2"], w=["sc10"])
        P.op("dve", "tensor_scalar", dict(out=fn32[:], in0=vec[:, 16:24], scalar1=32.0, scalar2=None, op0=ALU.mult),
             r=["vec"], w=["fn32"])

        P.op("act", "activation", dict(out=cact[:], in_=cact[:], func=AF.Silu), r=["cact"], w=["cact"])
        wst = [regC[:, 0:8192].bitcast(F32).rearrange("p (k n) -> p k n", n=512),
               regC[:, 8192:16384].bitcast(F32).rearrange("p (k n) -> p k n", n=512)]
        wadav = w_ada.rearrange("(k p) n -> p k n", p=128)
        ada_ps, ada_key = banks[0], ("bank", 0)
        ada_v = ada_ps[:, 0:48 * NBL].rearrange("p (f b) -> p f b", b=NBL)
        for blk in range(12):
            st = wst[blk % 2]
            P.dma("sp", st, wadav[:, :, blk * 512:(blk + 1) * 512], w=[("wst", blk % 2)])
            for fi in range(4):
                f = blk * 4 + fi
                for k in range(8):
                    P.op("pe", "matmul", dict(out=ada_v[:, f, :], lhsT=st[:, k, fi * 128:(fi + 1) * 128], rhs=cact[:, k, :],
                                               start=(k == 0), stop=(k == 7)),
                         r=[("wst", blk % 2), "cact"], w=[("adaps", f)], inc=(k == 7))
        P.op("dve", "tensor_tensor", dict(out=adaT[:], in0=ada_v, in1=bada[:].unsqueeze(2).to_broadcast([128, 48, NBL]),
                                           op=ALU.add), r=[("adaps", f) for f in range(48)] + ["bada"], w=["adaT"])
        for b in range(NBL):
            P.op("dve", "scalar_tensor_tensor", dict(out=modv[:, b, 0:8], in0=adaT[:, 8:16, b], scalar=1.0, in1=vec[:, 0:8],
                                                      op0=ALU.add, op1=ALU.mult), r=["adaT", "vec"], w=[("modv", b, 0)])
            P.op("dve", "scalar_tensor_tensor", dict(out=modv[:, b, 8:16], in0=adaT[:, 32:40, b], scalar=1.0, in1=vec[:, 8:16],
                                                      op0=ALU.add, op1=ALU.mult), r=["adaT", "vec"], w=[("modv", b, 1)])
        P.op("dve", "tensor_scalar", dict(out=modv[:], in0=modv[:], scalar1=32.0, scalar2=None, op0=ALU.mult),
             r=[("modv", b, i) for b in range(NBL) for i in range(2)], w=["modv"])
        if dbg:
            P.dma("sp", dbgs["d_ada"], adaT[:].rearrange("p f b -> p (f b)"), r=["adaT"])

        posS = regC[:, 16384:20480].bitcast(F32)
        tA = regC[:, 20480:24576].bitcast(F32)
        tB = regC[:, 24576:28672].bitcast(F32)
        P.dma("sp", posS, posr, w=["posS"])
        TWO_PI = 2.0 * math.pi
        MAGIC = 12582912.0
        tK = regC[:, 28672:32768].bitcast(F32)
        for mi in range(2):
            for ti, phase in ((0, math.pi / 2), (1, 0.0)):
                P.op("dve", "tensor_scalar", dict(out=tA, in0=posS, scalar1=smallc[:, mi:mi + 1], scalar2=phase,
                                                   op0=ALU.mult, op1=ALU.add), r=["posS", "sc0", "sc1"], w=["tA"])
                P.op("dve", "tensor_scalar", dict(out=tK, in0=tA, scalar1=1.0 / TWO_PI, scalar2=MAGIC,
                                                   op0=ALU.mult, op1=ALU.add), r=["tA"], w=["tK"])
                P.op("dve", "tensor_scalar", dict(out=tK, in0=tK, scalar1=-MAGIC, scalar2=None, op0=ALU.add), r=["tK"], w=["tK"])
                P.op("dve", "scalar_tensor_tensor", dict(out=tA, in0=tK, scalar=-TWO_PI, in1=tA, op0=ALU.mult, op1=ALU.add),
                     r=["tK", "tA"], w=["tA"])
                P.op("dve", "tensor_scalar", dict(out=tA, in0=tA, scalar1=3.14159, scalar2=-3.14159, op0=ALU.min, op1=ALU.max),
                     r=["tA"], w=["tA"])
                if ti == 0:
                    P.op("act", "activation", dict(out=tB, in_=tA, func=AF.Sin), r=["tA"], w=["tB"])
                else:
                    P.op("act", "activation", dict(out=tA, in_=tA, func=AF.Sin), r=["tA"], w=["tA"])
                    P.op("dve", "tensor_scalar", dict(out=tB, in0=tA, scalar1=idxc[:, 1:2], scalar2=None, op0=ALU.mult),
                         r=["tA", "cf"], w=["tB"])
                P.dma("sp", tabs[mi * 2 + ti], tB, r=["tB"], w=[("tabs", mi * 2 + ti)])
        P.barrier()
        stage = [0]

        def chk():
            stage[0] += 1
            if stop_after is not None and stage[0] >= stop_after:
                raise _Stop()

        hT = [regA[:, c * 2048:(c + 1) * 2048] for c in range(8)]
        aT = [regB[:, c * 2048:(c + 1) * 2048] for c in range(4)]
        gT = [regB[:, 8192 + h * 2048:8192 + (h + 1) * 2048] for h in range(8)]
        merged = [regC[:, f * 2048:(f + 1) * 2048] for f in range(8)]

        def gs(g):
            return slice(g * 512, (g + 1) * 512)

        def proj_fm(pbank, pkey, wblk, wkey, c0, g, rkeys):
            for k in range(8):
                P.op("pe", "matmul", dict(out=pbank[:], lhsT=wblk[:, k, c0:c0 + 128], rhs=hT[k][:, gs(g)],
                                           start=(k == 0), stop=(k == 7)),
                     r=[wkey] + rkeys, w=[pkey], inc=(k == 7))

        try:
          for b in range(NBL):
            chk()
            a1 = modv[:, b, 0:8]
            a2 = modv[:, b, 8:16]
            sh1 = adaT[:, 0:8, b]
            g1 = adaT[:, 16:24, b]
            sh2 = adaT[:, 24:32, b]
            g2 = adaT[:, 40:48, b]
            hkeys = [("hT", k, g) for k in range(8) for g in range(4)]

            xs = [regC[:, c * 4096:(c + 1) * 4096].bitcast(F32) for c in range(8)]
            sq = [regB[:, 0:2048], regB[:, 2048:4096]]
            rstd = regB[:, 4096:8192].bitcast(F32)
            tmpn = [regB[:, 8192:12288].bitcast(F32), regB[:, 12288:16384].bitcast(F32)]
            for c in range(8):
                P.dma("sp", xs[c], xT[b, c * 128:(c + 1) * 128, :], w=[("xs", c)])
            for c in range(8):
                P.op("act", "activation", dict(out=sq[c % 2], in_=xs[c], func=AF.Square), r=[("xs", c)], w=[("sq", c % 2)])
                for g in range(4):
                    P.op("pe", "matmul", dict(out=banks[g][:], lhsT=onesb[:], rhs=sq[c % 2][:, gs(g)], start=(c == 0), stop=(c == 7)),
                         r=[("sq", c % 2), "onesb"], w=[("bank", g)], inc=(g == 3))
            for g in range(4):
                P.op("act", "activation", dict(out=rstd[:, gs(g)], in_=banks[g][:], func=AF.Ln, bias=1024.0 * EPS, scale=1.0),
                     r=[("bank", g)], w=[("rstd", g)])
                P.op("act", "activation", dict(out=rstd[:, gs(g)], in_=rstd[:, gs(g)], func=AF.Exp, scale=-0.5),
                     r=[("rstd", g)], w=[("rstd", g)])
            for c in range(8):
                P.op("dve", "scalar_tensor_tensor", dict(out=tmpn[c % 2], in0=xs[c], scalar=a1[:, c:c + 1], in1=rstd,
                                                          op0=ALU.mult, op1=ALU.mult),
                     r=[("xs", c), "modv"] + [("rstd", g) for g in range(4)], w=[("tmpn", c % 2)])
                P.op("act", "activation", dict(out=hT[c], in_=tmpn[c % 2], func=AF.Identity, bias=sh1[:, c:c + 1], scale=1.0),
                     r=[("tmpn", c % 2), "adaT"], w=[("hT", c, g) for g in range(4)])
            if dbg and b == 0:
                for c in range(8):
                    P.dma("sp", dbgs["d_h"][c * 128:(c + 1) * 128, :], hT[c], r=[("hT", c, 0)])
            P.barrier()
            chk()

            cosm = regC[:, 0:4096].bitcast(F32)
            sinm = regC[:, 4096:8192].bitcast(F32)
            qT = regC[:, 8192:10240]
            kT = regC[:, 10240:12288]
            vaug = regC[:, 12288:16384].rearrange("p (t h e) -> p t h e", t=16, h=2)
            q_bf = regC[:, 16384:16896]
            t1 = regC[:, 16896:17920].bitcast(F32)
            t2 = regC[:, 17920:18944].bitcast(F32)
            gm = regC[:, 18944:19456].bitcast(F32)
            cmpb = regC[:, 19456:21504]
            cnt = regC[:, 21504:22016].bitcast(F32)
            sel = regC[:, 22016:22528].bitcast(F32)
            btok = regC[:, 22528:22784]
            biasT = regC[:, 22784:24832]
            pTr = [regC[:, 24832 + i * 512:24832 + (i + 1) * 512] for i in range(3)] + [regC[:, 27648:28160]]
            rs = regC[:, 26368:27392].bitcast(F32)
            km = regC[:, 27392:27408].bitcast(F32)
            kmb = regC[:, 27408:27416]
            P.dma("sp", cosm, tabs[0], r=[("tabs", 0)], w=["cosm"])
            P.dma("sp", sinm, tabs[1], r=[("tabs", 1)], w=["sinm"])
            P.op("dve", "memset", dict(ap=vaug[:, :, :, 64:128], constant=1.0), w=["vones"])
            P.op("dve", "memset", dict(ap=biasT, constant=0.0), w=[("biasT", g) for g in range(4)])

            def rotary(dst, dkey, wblk, wkey, c0, cosT, sinT, ckeys, decay=None):
                for g in range(4):
                    pq, pk_ = bank("mm", [0, 1])
                    proj_fm(pq, pk_, wblk, wkey, c0, g, hkeys)
                    P.op("act", "activation", dict(out=q_bf, in_=pq[:], func=AF.Copy), r=[pk_], w=["q_bf"])
                    psw, psk = bank("sw", [2, 3])
                    P.op("pe", "matmul", dict(out=psw[:], lhsT=pswb, rhs=q_bf, start=True, stop=True), r=["q_bf", "cb"], w=[psk])
                    P.op("dve", "tensor_tensor", dict(out=t1, in0=pq[:], in1=cosT[:, gs(g)], op=ALU.mult), r=[pk_] + ckeys, w=["t1"])
                    P.op("dve", "tensor_tensor", dict(out=t2, in0=psw[:], in1=sinT[:, gs(g)], op=ALU.mult), r=[psk] + ckeys, w=["t2"])
                    if decay is None:
                        P.op("dve", "tensor_tensor", dict(out=dst[:, gs(g)], in0=t1, in1=t2, op=ALU.add), r=["t1", "t2"], w=[(dkey, g)])
                    else:
                        decay(g, dst, dkey)

            for c in range(4):
                wblk, wkey = load_w("M%d" % c)
                chk()
                rotary(qT, "qT", wblk, wkey, 0, cosm, sinm, ["cosm", "sinm"])
                rotary(kT, "kT", wblk, wkey, 128, cosm, sinm, ["cosm", "sinm"])
                if dbg and b == 0 and c == 0:
                    P.dma("sp", dbgs["d_q"], qT, r=[("qT", g) for g in range(4)])
                    P.dma("sp", dbgs["d_k"], kT, r=[("kT", g) for g in range(4)])
                chk()
                for tq in range(4):
                    pv, pvk = bank("mm", [0, 1])
                    for tt in range(4):
                        t = tq * 4 + tt
                        for k in range(8):
                            P.op("pe", "matmul", dict(out=pv[:, tt * 128:(tt + 1) * 128], lhsT=hT[k][:, t * 128:(t + 1) * 128],
                                                       rhs=wblk[:, k, 256:384], start=(k == 0), stop=(k == 7)),
                                 r=[wkey] + hkeys, w=[pvk], inc=(k == 7 and tt == 3))
                    P.op("act", "activation", dict(out=vaug[:, tq * 4:(tq + 1) * 4, :, 0:64],
                                                    in_=pv[:].rearrange("p (t h e) -> p t h e", t=4, h=2), func=AF.Copy),
                         r=[pvk], w=[("vaug", tq)])
                chk()
                P.op("dve", "tensor_reduce", dict(out=km, in_=kT.rearrange("p (j t) -> p j t", t=256), axis=AX.X, op=ALU.add),
                     r=[("kT", g) for g in range(4)], w=["km"])
                P.op("dve", "tensor_scalar", dict(out=kmb, in0=km, scalar1=1.0 / 256.0, scalar2=None, op0=ALU.mult), r=["km"], w=["kmb"])
                chk()
                gpsl = [(banks[4], ("bank", 4)), (banks[5], ("bank", 5))]
                gmv = gm.rearrange("p (t h j) -> p t h j", t=16, h=2)
                pbv = pastbias.rearrange("p (t h j) -> p t h j", t=16, h=2)
                for hh in range(2):
                    gps, gk = gpsl[hh]
                    gv = gps[:, 0:128].rearrange("p (t j) -> p t j", j=8)
                    for t in range(16):
                        P.op("pe", "matmul", dict(out=gv[:, t, :], lhsT=qT[hh * 64:(hh + 1) * 64, t * 128:(t + 1) * 128],
                                                   rhs=kmb[hh * 64:(hh + 1) * 64, :], start=True, stop=True),
                             r=[("qT", t // 4), "kmb"], w=[gk], inc=(t == 15))
                    P.op("dve", "tensor_tensor", dict(out=gmv[:, :, hh, :], in0=gv, in1=pbv[:, :, hh, :], op=ALU.add),
                         r=[gk, "cf"], w=[("gm", hh)])
                chk()
                gm3 = gm.rearrange("p (g j) -> p g j", j=8)
                P.op("dve", "tensor_tensor", dict(out=cmpb.rearrange("p (g a c) -> p g a c", a=8, c=8),
                                                   in0=gm3.unsqueeze(2).to_broadcast([128, 32, 8, 8]),
                                                   in1=gm3.unsqueeze(3).to_broadcast([128, 32, 8, 8]), op=ALU.is_gt),
                     r=[("gm", 0), ("gm", 1)], w=["cmpb"])
                P.op("dve", "tensor_reduce", dict(out=cnt, in_=cmpb.rearrange("p (g c) -> p g c", c=8), axis=AX.X, op=ALU.add),
                     r=["cmpb"], w=["cnt"])
                P.op("dve", "scalar_tensor_tensor", dict(out=sel, in0=cnt, scalar=2.5, in1=past01, op0=ALU.is_lt, op1=ALU.mult),
                     r=["cnt", "cf"], w=["sel"])
                P.op("dve", "tensor_tensor", dict(out=sel, in0=sel, in1=own01, op=ALU.add), r=["sel", "cf"], w=["sel"])
                P.op("dve", "tensor_scalar", dict(out=btok, in0=sel, scalar1=-1.0, scalar2=BIG, op0=ALU.add, op1=ALU.mult),
                     r=["sel"], w=["btok"])
                chk()
                btv = btok.rearrange("p (t x) -> p t x", x=16)
                for g in range(4):
                    for tt in range(4):
                        P.op("pe", "transpose", dict(out=tpb[0:16, tt * 128:(tt + 1) * 128], in_=btv[:, g * 4 + tt, :], identity=identb),
                             r=["btok", "cb"], w=["tpb"], inc=(tt == 3))
                    P.op("act", "activation", dict(out=biasT[0:16, gs(g)], in_=tpb[0:16, 0:512], func=AF.Copy), r=["tpb"], w=[("biasT", g)])
                chk()
                tiles = []
                for hh in range(2):
                    for qg in range(4):
                        full = list(range(0, 4 * qg + 2))
                        part = [4 * qg + 2, 4 * qg + 3]
                        order = [full[0]] + part + full[1:]
                        for oi, kt in enumerate(order):
                            tiles.append((hh, qg, oi, kt, oi == 0, oi == len(order) - 1))
                LOOK = 2
                sb_ids = [2, 3, 0, 1]
                st = {}

                def s_stage(i):
                    hh, qg, oi, kt, first, last = tiles[i]
                    hs = slice(hh * 64, (hh + 1) * 64)
                    j = kt // 2
                    c0 = 256 if j == 2 * qg + 1 else 0
                    cols = slice(c0, 512)
                    qcols = slice(qg * 512 + c0, (qg + 1) * 512)
                    diag = (j >= 2 * qg)
                    bi = sb_ids[i % 4]
                    sps, spk = banks[bi], ("bank", bi)
                    P.op("pe", "matmul", dict(out=sps[:, cols], lhsT=kT[hs, kt * 128:(kt + 1) * 128], rhs=qT[hs, qcols],
                                               start=True, stop=False), r=[("kT", kt // 4), ("qT", qg)], w=[spk], inc=False)
                    P.op("pe", "matmul", dict(out=sps[:, cols], lhsT=Eselb[:, (hh * 8 + j) * 128:(hh * 8 + j + 1) * 128],
                                               rhs=biasT[:, qcols], start=False, stop=(not diag)),
                         r=[("biasT", qg), "cb"], w=[spk], inc=(not diag))
                    if diag:
                        oc = (j - 2 * qg) * 256
                        P.op("pe", "matmul", dict(out=sps[:, oc:oc + 256], lhsT=identb, rhs=B01b[:, (kt % 2) * 256:(kt % 2 + 1) * 256],
                                                   start=False, stop=True), r=["cb"], w=[spk])
                    st[i] = (sps, spk, cols)

                for i in range(min(LOOK, len(tiles))):
                    s_stage(i)
                acc = acck = None
                for i in range(len(tiles)):
                    hh, qg, oi, kt, first, last = tiles[i]
                    hs = slice(hh * 64, (hh + 1) * 64)
                    if i + LOOK < len(tiles):
                        s_stage(i + LOOK)
                    sps, spk, cols = st.pop(i)
                    if first:
                        acc, acck = bank("acc", [5, 6])
                    pT = pTr[i % 4]
                    pkey = ("pT", i % 4)
                    P.op("act", "activation", dict(out=pT[:, cols], in_=sps[:, cols], func=AF.Exp, scale=0.125), r=[spk], w=[pkey])
                    P.op("pe", "matmul", dict(out=acc[:, cols], lhsT=vaug[:, kt, hh, :], rhs=pT[:, cols], start=first, stop=last),
                         r=[pkey, ("vaug", kt // 4), "vones"], w=[acck])
                    if last:
                        P.op("dve", "reciprocal", dict(out=rs[0:64, :], in_=acc[64:128, :]), r=[acck], w=["rs"])
                        P.op("dve", "tensor_tensor", dict(out=aT[c][hs, gs(qg)], in0=acc[0:64, :], in1=rs[0:64, :], op=ALU.mult),
                             r=[acck, "rs"], w=[("aT", c, hh, qg)])
            if dbg and b == 0:
                for c in range(4):
                    P.dma("sp", dbgs["d_a"][c * 128:(c + 1) * 128, :], aT[c], r=[("aT", c, hh, qg) for hh in range(2) for qg in range(4)])
            P.barrier()
            chk()

            cosr = regC[:, 0:4096].bitcast(F32)
            sinr = regC[:, 4096:8192].bitcast(F32)
            posR = regC[:, 8192:12288].bitcast(F32)
            qd = regC[:, 12288:14336]
            kd = regC[:, 14336:16384]
            vtok = regC[:, 16384:20480].rearrange("p (t e) -> p t e", e=256)
            q_bf = regC[:, 20480:20992]
            t1 = regC[:, 20992:22016].bitcast(F32)
            t2 = regC[:, 22016:23040].bitcast(F32)
            dq = regC[:, 23040:24064].bitcast(F32)
            dk = regC[:, 24064:25088].bitcast(F32)
            ssum = regC[:, 25088:26112].bitcast(F32)
            Smr = [regC[:, 26112 + i * 128:26112 + (i + 1) * 128] for i in range(2)] + [regC[:, 31744 + i * 128:31744 + (i + 1) * 128] for i in range(2)]
            ktok_all = regD[:, 0:2048].rearrange("p (h x) -> p h x", h=2)
            Rbf_all = regD[:, 2048:4096]
            Rf = [regC[:, 26496 + i * 256:26496 + (i + 1) * 256].bitcast(F32) for i in range(2)]
            Rbf = regC[:, 27008:27136]
            obf = [regC[:, 27136 + i * 512:27136 + (i + 1) * 512] for i in range(2)]
            sqb = [regC[:, 28160 + i * 512:28160 + (i + 1) * 512] for i in range(2)]
            rstdn = regC[:, 29184:30208].bitcast(F32)
            yn = regC[:, 30208:31232].bitcast(F32)
            srg = regC[:, 31232:31744]
            P.dma("sp", cosr, tabs[2], r=[("tabs", 2)], w=["cosr"])
            P.dma("sp", sinr, tabs[3], r=[("tabs", 3)], w=["sinr"])
            P.dma("sp", posR, posr, w=["posR"])
            for c in range(4):
                wblk, wkey = load_w("R%d" % c)
                gblk, gkey = load_w("G%d" % c)

                def dec_q(g, dst, dkey, c=c):
                    P.op("act", "activation", dict(out=dq, in_=posR[:, gs(g)], func=AF.Exp, scale=smallc[:, 2 + c:3 + c],
                                                    bias=smallc[:, 2 + c:3 + c]), r=["posR", "sc2"], w=["dq"])
                    P.op("dve", "tensor_tensor", dict(out=ssum, in0=t1, in1=t2, op=ALU.add), r=["t1", "t2"], w=["ssum"])
                    P.op("dve", "tensor_tensor", dict(out=dst[:, gs(g)], in0=ssum, in1=dq, op=ALU.mult), r=["ssum", "dq"], w=[(dkey, g)])

                def dec_k(g, dst, dkey, c=c):
                    P.op("act", "activation", dict(out=dk, in_=posR[:, gs(g)], func=AF.Exp, scale=smallc[:, 6 + c:7 + c],
                                                    bias=smallc[:, 10 + c:11 + c]), r=["posR", "sc6", "sc10"], w=["dk"])
                    P.op("dve", "tensor_tensor", dict(out=ssum, in0=t1, in1=t2, op=ALU.add), r=["t1", "t2"], w=["ssum"])
                    P.op("dve", "tensor_tensor", dict(out=dst[:, gs(g)], in0=ssum, in1=dk, op=ALU.mult), r=["ssum", "dk"], w=[(dkey, g)])

                rotary(qd, "qd", wblk, wkey, 0, cosr, sinr, ["cosr", "sinr"], decay=dec_q)
                rotary(kd, "kd", wblk, wkey, 128, cosr, sinr, ["cosr", "sinr"], decay=dec_k)
                if dbg and b == 0 and c == 0:
                    P.dma("sp", dbgs["d_rq"], qd, r=[("qd", g) for g in range(4)])
                for t2_ in range(8):
                    pv, pvk = bank("mm", [0, 1])
                    for tt in range(2):
                        t = t2_ * 2 + tt
                        for k in range(8):
                            P.op("pe", "matmul", dict(out=pv[:, tt * 256:(tt + 1) * 256], lhsT=hT[k][:, t * 128:(t + 1) * 128],
                                                       rhs=wblk[:, k, 256:512], start=(k == 0), stop=(k == 7)),
                                 r=[wkey] + hkeys, w=[pvk], inc=(k == 7 and tt == 1))
                    P.op("act", "activation", dict(out=vtok[:, t2_ * 2:(t2_ + 1) * 2, :],
                                                    in_=pv[:].rearrange("p (t e) -> p t e", e=256), func=AF.Copy),
                         r=[pvk], w=[("vtok", t2_)])
                P.op("dve", "memset", dict(ap=Rf[0][0:64, :], constant=0.0), w=[("Rf", 0)])
                P.op("dve", "memset", dict(ap=Rf[1][0:64, :], constant=0.0), w=[("Rf", 1)])
                for hh in range(2):
                    hs = slice(hh * 64, (hh + 1) * 64)
                    for n4 in range(4):
                        nn = [n for n in range(4 * n4, 4 * n4 + 4) if n < 15]
                        for i, n in enumerate(nn):
                            P.op("pe", "transpose", dict(out=tpb[:, i * 64:(i + 1) * 64], in_=kd[hs, n * 128:(n + 1) * 128], identity=identb[hs, hs]),
                                 r=[("kd", n // 4), "cb"], w=["tpb"], inc=(i == len(nn) - 1))
                        P.op("act", "activation", dict(out=ktok_all[:, hh, n4 * 256:n4 * 256 + 64 * len(nn)], in_=tpb[:, 0:64 * len(nn)], func=AF.Copy),
                             r=["tpb"], w=[("ktok", hh, n4)])
                for n4 in range(4):
                    for hh in range(2):
                        hs = slice(hh * 64, (hh + 1) * 64)
                        nn = [n for n in range(4 * n4, 4 * n4 + 4) if n < 15]
                        kb = [4, 5][hh]
                        kvp, kvk = banks[kb], ("bank", kb)
                        for i, n in enumerate(nn):
                            P.op("pe", "matmul", dict(out=kvp[0:64, i * 128:(i + 1) * 128], lhsT=ktok_all[:, hh, n * 64:(n + 1) * 64],
                                                       rhs=vtok[:, n, hh * 128:(hh + 1) * 128], start=True, stop=True),
                                 r=[("ktok", hh, n4), ("vtok", n // 2)], w=[kvk], inc=(i == len(nn) - 1))
                        for i, n in enumerate(nn):
                            P.op("dve", "tensor_tensor", dict(out=Rf[hh][0:64, :], in0=kvp[0:64, i * 128:(i + 1) * 128], in1=Rf[hh][0:64, :], op=ALU.add),
                                 r=[kvk, ("Rf", hh)], w=[("Rf", hh)])
                            P.op("act", "activation", dict(out=Rbf_all[hs, (n + 1) * 128:(n + 2) * 128], in_=Rf[hh][0:64, :], func=AF.Copy),
                                 r=[("Rf", hh)], w=[("Rbf", hh, n + 1)])
                steps = [(n, hh) for n in range(16) for hh in range(2)]
                sst = {}

                def s_step(i):
                    n, hh = steps[i]
                    hs = slice(hh * 64, (hh + 1) * 64)
                    ncol = slice(n * 128, (n + 1) * 128)
                    bi = [2, 3][i % 2]
                    sps, spk = banks[bi], ("bank", bi)
                    P.op("pe", "matmul", dict(out=sps[:, 0:128], lhsT=kd[hs, ncol], rhs=qd[hs, ncol], start=True, stop=True),
                         r=[("kd", n // 4), ("qd", n // 4)], w=[spk])
                    sst[i] = (sps, spk)

                s_step(0)
                ops_ = {}
                for i, (n, hh) in enumerate(steps):
                    g = n // 4
                    ncol = slice(n * 128, (n + 1) * 128)
                    h = 2 * c + hh
                    hs = slice(hh * 64, (hh + 1) * 64)
                    if i + 1 < len(steps):
                        s_step(i + 1)
                    if n % 4 == 0:
                        ops_[hh] = bank("o", [5, 6]) if False else ((banks[5], ("bank", 5)) if hh == 0 else (banks[6], ("bank", 6)))
                    o_ps, ok = ops_[hh]
                    ocol = slice((n % 4) * 128, (n % 4 + 1) * 128)
                    sps, spk = sst.pop(i)
                    Sm = Smr[i % 4]
                    smk = ("Sm", i % 4)
                    P.op("dve", "tensor_tensor", dict(out=Sm, in0=sps[:, 0:128], in1=tri, op=ALU.mult), r=[spk, "cf"], w=[smk])
                    P.op("pe", "matmul", dict(out=o_ps[:, ocol], lhsT=vtok[:, n, hh * 128:(hh + 1) * 128], rhs=Sm, start=True, stop=(n == 0)),
                         r=[smk, ("vtok", n // 2)], w=[ok], inc=(n == 0))
                    if n > 0:
                        P.op("pe", "matmul", dict(out=o_ps[:, ocol], lhsT=Rbf_all[hs, n * 128:(n + 1) * 128], rhs=qd[hs, ncol], start=False, stop=True),
                             r=[("Rbf", hh, n), ("qd", g)], w=[ok])
                    if n % 4 == 3:
                        ob = obf[hh]
                        P.op("act", "activation", dict(out=ob, in_=o_ps[:], func=AF.Copy), r=[ok], w=[("obf", hh)])
                        cen, cenk = bank("mm", [0, 1])
                        P.op("pe", "matmul", dict(out=cen[:], lhsT=cmatb[:], rhs=ob, start=True, stop=True), r=[("obf", hh), "cmatb"], w=[cenk])
                        sq_ = sqb[hh]
                        P.op("act", "activation", dict(out=sq_, in_=cen[:], func=AF.Square), r=[cenk], w=[("sqb", hh)])
                        var, vark = banks[4], ("bank", 4)
                        P.op("pe", "matmul", dict(out=var[:], lhsT=o128b[:], rhs=sq_, start=True, stop=True), r=[("sqb", hh), "o128b"], w=[vark])
                        P.op("act", "activation", dict(out=rstdn, in_=var[:], func=AF.Ln, bias=EPS, scale=1.0), r=[vark], w=["rstdn"])
                        P.op("act", "activation", dict(out=rstdn, in_=rstdn, func=AF.Exp, scale=-0.5), r=["rstdn"], w=["rstdn"])
                        P.op("dve", "tensor_tensor", dict(out=yn, in0=cen[:], in1=rstdn, op=ALU.mult), r=[cenk, "rstdn"], w=["yn"])
                        prg, prgk = bank("mm", [0, 1])
                        proj_fm(prg, prgk, gblk, gkey, hh * 128, g, hkeys)
                        P.op("act", "activation", dict(out=srg, in_=prg[:], func=AF.Silu), r=[prgk], w=["srg"])
                        P.op("dve", "scalar_tensor_tensor", dict(out=gT[h][:, gs(g)], in0=yn, scalar=vec[:, 24 + h:25 + h], in1=srg,
                                                                  op0=ALU.mult, op1=ALU.mult), r=["yn", "srg", "vec"], w=[("gT", h, g)])
            if dbg and b == 0:
                for h in range(8):
                    P.dma("sp", dbgs["d_g"][h * 128:(h + 1) * 128, :], gT[h], r=[("gT", h, g) for g in range(4)])
            P.barrier()
            chk()

            sgr = [regC[:, 16384 + i * 512:16384 + (i + 1) * 512] for i in range(2)]
            tmpo = [regC[:, 17408 + i * 1024:17408 + (i + 1) * 1024].bitcast(F32) for i in range(2)]
            it = 0
            for f in range(8):
                if f % 4 == 0:
                    wmo, wmok = load_w("MO%d" % (f // 4))
                    gab, gabk = load_w("GA%d" % (f // 4))
                for g in range(4):
                    pga, pgak = bank("mm", [0, 1])
                    proj_fm(pga, pgak, gab, gabk, (f % 4) * 128, g, hkeys)
                    sg = sgr[it % 2]
                    P.op("act", "activation", dict(out=sg, in_=pga[:], func=AF.Sigmoid), r=[pgak], w=[("sgr", it % 2)])
                    pya, pyak = bank("S", [2, 3])
                    for cc in range(4):
                        P.op("pe", "matmul", dict(out=pya[:], lhsT=wmo[:, cc, (f % 4) * 128:(f % 4 + 1) * 128], rhs=aT[cc][:, gs(g)],
                                                   start=(cc == 0), stop=(cc == 3)),
                             r=[wmok] + [("aT", cc, hh, g) for hh in range(2)], w=[pyak], inc=(cc == 3))
                    P.op("dve", "tensor_tensor", dict(out=merged[f][:, gs(g)], in0=pya[:], in1=sg, op=ALU.mult),
                         r=[pyak, ("sgr", it % 2)], w=[("merged", f, g)])
                    it += 1
            for f in range(8):
                if f % 4 == 0:
                    grb, grbk = load_w("GR%d" % (f // 4))
                    rob, robk = load_w("RO%d" % (f // 4))
                for g in range(4):
                    pga, pgak = bank("mm", [0, 1])
                    proj_fm(pga, pgak, grb, grbk, (f % 4) * 128, g, hkeys)
                    sg = sgr[it % 2]
                    P.op("act", "activation", dict(out=sg, in_=pga[:], func=AF.Sigmoid), r=[pgak], w=[("sgr", it % 2)])
                    pyr, pyrk = bank("S", [2, 3])
                    for h in range(8):
                        P.op("pe", "matmul", dict(out=pyr[:], lhsT=rob[:, h, (f % 4) * 128:(f % 4 + 1) * 128], rhs=gT[h][:, gs(g)],
                                                   start=(h == 0), stop=(h == 7)), r=[robk, ("gT", h, g)], w=[pyrk], inc=(h == 7))
                    tm = tmpo[it % 2]
                    P.op("dve", "tensor_tensor", dict(out=tm, in0=pyr[:], in1=sg, op=ALU.mult), r=[pyrk, ("sgr", it % 2)], w=[("tmpo", it % 2)])
                    P.op("dve", "tensor_tensor", dict(out=merged[f][:, gs(g)], in0=merged[f][:, gs(g)], in1=tm, op=ALU.add),
                         r=[("merged", f, g), ("tmpo", it % 2)], w=[("merged", f, g)])
                    it += 1
            if dbg and b == 0:
                for f in range(8):
                    P.dma("sp", dbgs["d_m"][f * 128:(f + 1) * 128, :], merged[f], r=[("merged", f, g) for g in range(4)])
            P.barrier()
            chk()

            x1 = regA[:, 0:8192].bitcast(F32).rearrange("p (f t) -> p f t", t=512)
            h2 = regA[:, 8192:12288].rearrange("p (f t) -> p f t", t=512)
            sq2 = [regA[:, 12288 + i * 512:12288 + (i + 1) * 512] for i in range(2)]
            rstd2 = regA[:, 13312:14336].bitcast(F32)
            tmp2 = [regA[:, 14336 + i * 1024:14336 + (i + 1) * 1024].bitcast(F32) for i in range(2)]
            u = regB[:, 0:16384].rearrange("p (f t) -> p f t", t=512)
            rl = [regB[:, 16384 + i * 512:16384 + (i + 1) * 512] for i in range(2)]
            xTv = xT[b].rearrange("(f p) t -> p f t", p=128)
            oTv = outT[b].rearrange("(f p) t -> p f t", p=128)

            def rms_stats(src_keys):
                ssp, ssk = banks[4], ("bank", 4)
                for f in range(8):
                    P.op("act", "activation", dict(out=sq2[f % 2], in_=x1[:, f, :], func=AF.Square), r=[src_keys[f]], w=[("sq2", f % 2)])
                    P.op("pe", "matmul", dict(out=ssp[:], lhsT=onesb[:], rhs=sq2[f % 2], start=(f == 0), stop=(f == 7)),
                         r=[("sq2", f % 2), "onesb"], w=[ssk])
                P.op("act", "activation", dict(out=rstd2, in_=ssp[:], func=AF.Ln, bias=1024.0 * EPS, scale=1.0), r=[ssk], w=["rstd2"])
                P.op("act", "activation", dict(out=rstd2, in_=rstd2, func=AF.Exp, scale=-0.5), r=["rstd2"], w=["rstd2"])

            for g in range(4):
                P.dma("sp", x1, xTv[:, :, gs(g)], w=[("x1", f) for f in range(8)])
                for f in range(8):
                    if f % 4 == 0:
                        wob, wobk = load_w("WO%d" % (f // 4))
                    pm, pmk = bank("mm", [0, 1])
                    for cc in range(8):
                        P.op("pe", "matmul", dict(out=pm[:], lhsT=wob[:, cc, (f % 4) * 128:(f % 4 + 1) * 128], rhs=merged[cc][:, gs(g)],
                                                   start=(cc == 0), stop=(cc == 7)), r=[wobk, ("merged", cc, g)], w=[pmk], inc=(cc == 7))
                    P.op("dve", "scalar_tensor_tensor", dict(out=x1[:, f, :], in0=pm[:], scalar=g1[:, f:f + 1], in1=x1[:, f, :],
                                                              op0=ALU.mult, op1=ALU.add), r=[pmk, ("x1", f), "adaT"], w=[("x1", f)])
                if dbg and b == 0 and g == 0:
                    P.dma("sp", dbgs["d_x1"].rearrange("(f p) t -> p f t", p=128), x1, r=[("x1", f) for f in range(8)])
                rms_stats([("x1", f) for f in range(8)])
                for f in range(8):
                    P.op("dve", "scalar_tensor_tensor", dict(out=tmp2[f % 2], in0=x1[:, f, :], scalar=a2[:, f:f + 1], in1=rstd2,
                                                              op0=ALU.mult, op1=ALU.mult), r=[("x1", f), "rstd2", "modv"], w=[("tmp2", f % 2)])
                    P.op("act", "activation", dict(out=h2[:, f, :], in_=tmp2[f % 2], func=AF.Identity, bias=sh2[:, f:f + 1], scale=1.0),
                         r=[("tmp2", f % 2), "adaT"], w=[("h2", f)])
                for ffc in range(32):
                    if ffc % 4 == 0:
                        f1b, f1k = load_w("F1_%d" % (ffc // 4))
                    pu, puk = bank("mm", [0, 1])
                    for k in range(8):
                        P.op("pe", "matmul", dict(out=pu[:], lhsT=f1b[:, k, (ffc % 4) * 128:(ffc % 4 + 1) * 128], rhs=h2[:, k, :],
                                                   start=(k == 0), stop=(k == 7)), r=[f1k, ("h2", k)], w=[puk], inc=(k == 7))
                    P.op("act", "activation", dict(out=rl[ffc % 2], in_=pu[:], func=AF.Relu), r=[puk], w=[("rl", ffc % 2)])
                    P.op("dve", "tensor_tensor", dict(out=u[:, ffc, :], in0=rl[ffc % 2], in1=rl[ffc % 2], op=ALU.mult),
                         r=[("rl", ffc % 2)], w=[("u", ffc)])
                for f in range(8):
                    f2b, f2k = load_w("F2_%d" % f)
                    pf, pfk = bank("S", [2, 3])
                    for ffc in range(32):
                        P.op("pe", "matmul", dict(out=pf[:], lhsT=f2b[:, ffc, :], rhs=u[:, ffc, :], start=(ffc == 0), stop=(ffc == 31)),
                             r=[f2k, ("u", ffc)], w=[pfk], inc=(ffc == 31))
                    P.op("dve", "scalar_tensor_tensor", dict(out=x1[:, f, :], in0=pf[:], scalar=g2[:, f:f + 1], in1=x1[:, f, :],
                                                              op0=ALU.mult, op1=ALU.add), r=[pfk, ("x1", f), "adaT"], w=[("x1", f)])
                rms_stats([("x1", f) for f in range(8)])
                for f in range(8):
                    P.op("dve", "scalar_tensor_tensor", dict(out=x1[:, f, :], in0=x1[:, f, :], scalar=fn32[:, f:f + 1], in1=rstd2,
                                                              op0=ALU.mult, op1=ALU.mult), r=[("x1", f), "rstd2", "fn32"], w=[("x1", f)])
                P.dma("pool", oTv[:, :, gs(g)], x1, r=[("x1", f) for f in range(8)], w=[("out", b, g)])
            P.barrier()
            chk()

        except _Stop:
            for e in P.CENG:
                for pt in P.pending[e]:
                    pt.val = P.cnt[e] + 1
            P.barrier_soft = True

        P.wait_all("sp", P.bar_toks)
        for e in ("sp", "pool"):
            for slot in P.dsem[e]:
                if slot[2] is not None:
                    P.wait_all("sp", [slot[2]])
        P.emit()
    return nc


def structural_constants():
    p = np.arange(128)
    cb = np.zeros((128, 2816), np.float32)
    cb[p, p] = 1.0
    sw = (p // 64) * 64 + ((p % 64) + 32) % 64
    cb[p, 128 + sw] = 1.0
    cc = np.arange(256)
    cb[:, 256:512] = np.where(cc[None, :] >= p[:, None], 0.0, -BIG)
    cb[:, 512:768] = np.where(cc[None, :] - 128 >= p[:, None], 0.0, -BIG)
    for r in range(16):
        cb[r, 768 + r * 128:768 + (r + 1) * 128] = 1.0
    cf = np.zeros((128, 904), np.float32)
    cf[:, 0:128] = (p[:, None] <= p[None, :]).astype(np.float32)
    t = np.arange(16)[:, None, None]
    j = np.arange(8)[None, None, :]
    past = np.broadcast_to(j < (t // 2), (16, 2, 8))
    own = np.broadcast_to(j == (t // 2), (16, 2, 8))
    cf[:, 128:384] = np.where(past, 0.0, -1e30).reshape(1, 256)
    cf[:, 384:640] = past.astype(np.float32).reshape(1, 256)
    cf[:, 640:896] = own.astype(np.float32).reshape(1, 256)
    cf[:, 896] = p % 32
    cf[:, 897] = np.where((p % 64) < 32, -1.0, 1.0)
    for c in range(4):
        cf[:, 898 + c] = 2 * c + p // 64
    pos = np.broadcast_to(np.arange(S, dtype=np.float32)[None, :], (128, S)).copy()
    return cb, cf, pos


_CACHE = {}


def make_in_maps(inputs, nbl, cores):
    f32 = np.float32
    x = np.asarray(inputs["x"], f32)
    c = np.asarray(inputs["c"], f32)
    cb, cf, pos = structural_constants()
    vecs = np.concatenate([np.asarray(inputs[k], f32).reshape(8, 128).T for k in ("ln1_w", "ln2_w", "final_norm_w", "ret_gn_w")], axis=1)
    shared = {
        "w_ada": np.ascontiguousarray(np.asarray(inputs["w_ada"], f32)[0]),
        "badaT": np.ascontiguousarray(np.asarray(inputs["b_ada"], f32)[0].reshape(48, 128).T),
        "vecs": np.ascontiguousarray(vecs),
        "w_in": np.ascontiguousarray(np.asarray(inputs["w_in"], f32)[0]),
        "w_moba_o": np.ascontiguousarray(np.asarray(inputs["w_moba_o"], f32)[0]),
        "w_ret_o": np.ascontiguousarray(np.asarray(inputs["w_ret_o"], f32)[0]),
        "w_out": np.ascontiguousarray(np.asarray(inputs["w_out"], f32)[0]),
        "w_ff1": np.ascontiguousarray(np.asarray(inputs["w_ff1"], f32)[0]),
        "w_ff2": np.ascontiguousarray(np.asarray(inputs["w_ff2"], f32)[0]),
        "cstb": cb, "cstf": cf, "posr": pos,
    }
    maps = []
    for i in range(cores):
        xb = x[i * nbl:(i + 1) * nbl]
        m = dict(shared)
        m["xT"] = np.ascontiguousarray(xb.transpose(0, 2, 1))
        cbt = c[i * nbl:(i + 1) * nbl]
        m["cT"] = np.ascontiguousarray(cbt.T.reshape(8, 128, nbl).transpose(1, 0, 2))
        maps.append(m)
    return maps


def kernel(**inputs):
    B = inputs["x"].shape[0]
    nbl = B // NCORES
    if nbl not in _CACHE:
        _CACHE[nbl] = build(nbl)
    nc = _CACHE[nbl]
    maps = make_in_maps(inputs, nbl, NCORES)
    res = run_bass_kernel_spmd(nc, maps, core_ids=list(range(NCORES)))
    outs = [np.asarray(r["outT"]).transpose(0, 2, 1) for r in res.results]
    return np.ascontiguousarray(np.concatenate(outs, axis=0).astype(np.float32))
```
